# Optimizing a Trainium2 kernel written in Bass

```python
import math
import jax
import jax.numpy as jnp
from jax import lax
import numpy as np

D_MODEL = 2048
BATCH = 8
SEQ = 2048
DEPTH = 2

GRID_W = 64
CTX_LEN = 256
EPS = 1e-6

A_HEADS = 8
A_DK = 128
A_DV = 128
A_WIDTH = A_HEADS * A_DK
HGRN_CHUNK = 32

B_HEADS = 8
B_DH = 64
B_DV = 2 * B_DH
B_QK_WIDTH = B_HEADS * 2 * B_DH
B_WIDTH = B_HEADS * B_DV
Q_BLOCK = 128
ROPE_BASE = 10000.0

EVEN_SPLITS = (A_WIDTH, A_WIDTH, A_WIDTH, A_WIDTH, A_WIDTH, B_QK_WIDTH, B_QK_WIDTH, B_WIDTH)
EVEN_IN = 5 * A_WIDTH + 2 * B_QK_WIDTH + B_WIDTH
MIX_OUT = A_WIDTH + B_WIDTH

RG_WIDTH = D_MODEL
RG_HEADS = 16
RG_BLOCK = RG_WIDTH // RG_HEADS
CONV_W = 4
RG_C = 8.0

N_GROUPS = 4
EXPERTS_PER_GROUP = 4
N_EXPERTS = N_GROUPS * EXPERTS_PER_GROUP
TOPK_IN_GROUP = 2
D_EXPERT = 512

kernel_name = 'hybrid_hgrn2_diffattn_rglru_hmoe_dit'


def rms_norm(x, gain):
    xf = x.astype(jnp.float32)
    y = xf * lax.rsqrt(jnp.mean(xf * xf, axis=-1, keepdims=True) + EPS)
    return (y * gain.astype(jnp.float32)).astype(x.dtype)


def modulate(h, shift, scale):
    return h * (1.0 + scale) + shift


def ada(cvec, w, b):
    return jax.nn.silu(cvec) @ w + b


def to_heads(t, n_heads):
    b, n, w = t.shape
    return t.reshape(b, n, n_heads, w // n_heads).transpose(0, 2, 1, 3)


def merge_heads(t):
    b, h, n, d = t.shape
    return t.transpose(0, 2, 1, 3).reshape(b, n, h * d)


def axial_rope(n):
    n_rows = n // GRID_W
    row = jnp.repeat(jnp.arange(n_rows), GRID_W).astype(jnp.float32)
    col = jnp.tile(jnp.arange(GRID_W), n_rows).astype(jnp.float32)
    pairs = B_DH // 4
    inv = ROPE_BASE ** (-jnp.arange(pairs, dtype=jnp.float32) / pairs)
    ang = jnp.concatenate([row[:, None] * inv, col[:, None] * inv], axis=-1)
    return jnp.cos(ang)[:, None, None, :], jnp.sin(ang)[:, None, None, :]


def apply_rope(t, cos, sin):
    tp = t.reshape(t.shape[:-1] + (-1, 2))
    t0, t1 = tp[..., 0], tp[..., 1]
    out = jnp.stack([t0 * cos - t1 * sin, t0 * sin + t1 * cos], axis=-1)
    return out.reshape(t.shape).astype(t.dtype)


def diff_qk(t, gain, rope):
    b, n, _ = t.shape
    t = rms_norm(t.reshape(b, n, B_HEADS, 2, B_DH), gain)
    if rope is not None:
        t = apply_rope(t, rope[0], rope[1])
    return t.transpose(0, 2, 3, 1, 4)


def diff_attention(q, k, v, lam):
    s = jnp.einsum('bhmqd,bhmkd->bhmqk', q, k).astype(jnp.float32) * (B_DH ** -0.5)
    p = jax.nn.softmax(s, axis=-1)
    w = p[:, :, 0] - lam * p[:, :, 1]
    return jnp.einsum('bhqk,bhkv->bhqv', w.astype(v.dtype), v)


def diff_attention_blocked(q, k, v, lam):
    b, h, _, n, dh = q.shape
    nb = n // Q_BLOCK
    qb = jnp.moveaxis(q.reshape(b, h, 2, nb, Q_BLOCK, dh), 3, 0)
    ob = lax.map(lambda blk: diff_attention(blk, k, v, lam), qb)
    return jnp.moveaxis(ob, 0, 2).reshape(b, h, n, -1)


def hgrn2_scan(q, k, v, log_f, s0):
    b, h, n, dk = q.shape
    dv = v.shape[-1]
    nc = n // HGRN_CHUNK

    def to_chunks(t):
        return jnp.moveaxis(t.reshape(b, h, nc, HGRN_CHUNK, t.shape[-1]), 2, 0)

    mask = jnp.tril(jnp.ones((HGRN_CHUNK, HGRN_CHUNK), dtype=bool))

    def step(s, xs):
        qc, kc, vc, lf = xs
        cum = jnp.cumsum(lf, axis=2)
        cum_last = cum[:, :, -1:, :]
        q_t = qc * jnp.exp(cum)
        k_t = kc * jnp.exp(-cum)
        att = jnp.where(mask, jnp.einsum('bhtk,bhsk->bhts', q_t, k_t), 0.0)
        o = jnp.einsum('bhts,bhsv->bhtv', att, vc) + jnp.einsum('bhtk,bhkv->bhtv', q_t, s)
        s_new = jnp.exp(cum_last[:, :, 0, :])[..., None] * s + jnp.einsum('bhsk,bhsv->bhkv', kc * jnp.exp(cum_last - cum), vc)
        return s_new, o

    s_fin, o = lax.scan(step, s0, (to_chunks(q), to_chunks(k), to_chunks(v), to_chunks(log_f)))
    return jnp.moveaxis(o, 0, 2).reshape(b, h, n, dv), s_fin


def hgrn2_dir(q, v, f_logit, lb, s0, reverse):
    f = lb + (1.0 - lb) * jax.nn.sigmoid(f_logit)
    k = to_heads(1.0 - f, A_HEADS)
    log_f = to_heads(jnp.log(f), A_HEADS)
    if reverse:
        q, k, v, log_f = (jnp.flip(t, axis=2) for t in (q, k, v, log_f))
    o, s = hgrn2_scan(q, k, v, log_f, s0)
    if reverse:
        o = jnp.flip(o, axis=2)
    return o, s


def linear_scan(a, bx, h0):
    bx = bx.at[:, 0].add(a[:, 0] * h0)

    def comb(e1, e2):
        a1, b1 = e1
        a2, b2 = e2
        return a1 * a2, a2 * b1 + b2

    _, h = lax.associative_scan(comb, (a, bx), axis=1)
    return h, h[:, -1]


def rglru_dir(u, gate_w, gate_b, lam, h0, reverse):
    b, n, _ = u.shape
    uf = u.astype(jnp.float32)
    gates = jnp.einsum('bthi,ghij->gbthj', uf.reshape(b, n, RG_HEADS, RG_BLOCK), gate_w.astype(jnp.float32))
    gates = gates.reshape(2, b, n, RG_WIDTH) + gate_b[:, None, None, :]
    r = jax.nn.sigmoid(gates[0])
    i = jax.nn.sigmoid(gates[1])
    log_a = -RG_C * jax.nn.softplus(-lam.astype(jnp.float32)) * r
    a = jnp.exp(log_a)
    bx = jnp.sqrt(-jnp.expm1(2.0 * log_a)) * (i * uf)
    if reverse:
        a, bx = jnp.flip(a, axis=1), jnp.flip(bx, axis=1)
    h, h_last = linear_scan(a, bx, h0)
    if reverse:
        h = jnp.flip(h, axis=1)
    return h, h_last


def dwconv(u, w, b):
    out = lax.conv_general_dilated(u, w[:, None, :].astype(u.dtype), window_strides=(1,),
                                   padding=[(CONV_W // 2, CONV_W - 1 - CONV_W // 2)],
                                   dimension_numbers=('NWC', 'WIO', 'NWC'), feature_group_count=u.shape[-1])
    return out + b


def even_mixer(h_lat, h_ctx, w_in, w_out, lb_f, lb_b, hgrn_gain, qk_gain, lam_vec, diff_gain, lam_init, rope, with_ctx):
    idx = np.cumsum(EVEN_SPLITS)[:-1].tolist()
    pl = jnp.split(h_lat @ w_in, idx, axis=-1)
    pc = jnp.split(h_ctx @ w_in, idx, axis=-1)
    bsz = h_lat.shape[0]

    def a_prep(parts):
        q = to_heads(jax.nn.silu(parts[0].astype(jnp.float32)), A_HEADS)
        v = to_heads(parts[3].astype(jnp.float32), A_HEADS)
        return q, v, parts[1].astype(jnp.float32), parts[2].astype(jnp.float32)

    def a_out(o, g):
        return merge_heads(rms_norm(o, hgrn_gain)).astype(g.dtype) * jax.nn.silu(g)

    s0 = jnp.zeros((bsz, A_HEADS, A_DK, A_DV), jnp.float32)
    qc, vc, ffc, fbc = a_prep(pc)
    oc_f, sc_f = hgrn2_dir(qc, vc, ffc, lb_f, s0, False)
    oc_b, sc_b = hgrn2_dir(qc, vc, fbc, lb_b, s0, True)
    ql, vl, ffl, fbl = a_prep(pl)
    ol_f, _ = hgrn2_dir(ql, vl, ffl, lb_f, sc_f, False)
    ol_b, _ = hgrn2_dir(ql, vl, fbl, lb_b, sc_b, True)

    lv = lam_vec.astype(jnp.float32)
    lam = jnp.exp(jnp.sum(lv[0] * lv[1])) - jnp.exp(jnp.sum(lv[2] * lv[3])) + lam_init
    q_l = diff_qk(pl[5], qk_gain[0], rope)
    k_l = diff_qk(pl[6], qk_gain[1], rope)
    v_l = to_heads(pl[7], B_HEADS)
    k_c = diff_qk(pc[6], qk_gain[1], None)
    v_c = to_heads(pc[7], B_HEADS)
    k_all = jnp.concatenate([k_l, k_c], axis=3)
    v_all = jnp.concatenate([v_l, v_c], axis=2)
    ob_l = rms_norm(diff_attention_blocked(q_l, k_all, v_all, lam), diff_gain) * (1.0 - lam_init)
    out_lat = jnp.concatenate([a_out(ol_f + ol_b, pl[4]), merge_heads(ob_l).astype(h_lat.dtype)], axis=-1) @ w_out
    if not with_ctx:
        return out_lat, None
    q_c = diff_qk(pc[5], qk_gain[0], None)
    ob_c = rms_norm(diff_attention(q_c, k_c, v_c, lam), diff_gain) * (1.0 - lam_init)
    out_ctx = jnp.concatenate([a_out(oc_f + oc_b, pc[4]), merge_heads(ob_c).astype(h_ctx.dtype)], axis=-1) @ w_out
    return out_lat, out_ctx


def odd_mixer(h_lat, h_ctx, w_in, conv_w, conv_b, gate_w, gate_b, lam, w_out, with_ctx):
    y_l, u_l = jnp.split(h_lat @ w_in, 2, axis=-1)
    y_c, u_c = jnp.split(h_ctx @ w_in, 2, axis=-1)
    u_l = dwconv(u_l, conv_w, conv_b)
    u_c = dwconv(u_c, conv_w, conv_b)
    h0 = jnp.zeros((h_lat.shape[0], RG_WIDTH), jnp.float32)
    hc_f, sc_f = rglru_dir(u_c, gate_w[0], gate_b[0], lam[0], h0, False)
    hc_b, sc_b = rglru_dir(u_c, gate_w[1], gate_b[1], lam[1], h0, True)
    hl_f, _ = rglru_dir(u_l, gate_w[0], gate_b[0], lam[0], sc_f, False)
    hl_b, _ = rglru_dir(u_l, gate_w[1], gate_b[1], lam[1], sc_b, True)
    out_lat = ((hl_f + hl_b).astype(y_l.dtype) * jax.nn.gelu(y_l)) @ w_out
    if not with_ctx:
        return out_lat, None
    out_ctx = ((hc_f + hc_b).astype(y_c.dtype) * jax.nn.gelu(y_c)) @ w_out
    return out_lat, out_ctx


def hier_moe(h, w_grp, b_grp, w_exp, b_exp, w1, w3, w2):
    n_tok = h.shape[0]
    g_logit = (h @ w_grp).astype(jnp.float32) + b_grp
    g_prob = jax.nn.softmax(g_logit, axis=-1)
    g_idx = jnp.argmax(g_logit, axis=-1)
    g_w = jnp.take_along_axis(g_prob, g_idx[:, None], axis=-1)
    e_logit = ((h @ w_exp).astype(jnp.float32) + b_exp).reshape(n_tok, N_GROUPS, EXPERTS_PER_GROUP)
    e_logit = jnp.take_along_axis(e_logit, g_idx[:, None, None], axis=1)[:, 0]
    top_v, top_i = lax.top_k(e_logit, TOPK_IN_GROUP)
    top_w = jax.nn.softmax(top_v, axis=-1) * g_w
    expert_id = g_idx[:, None] * EXPERTS_PER_GROUP + top_i
    combine = jnp.einsum('nk,nke->ne', top_w, jax.nn.one_hot(expert_id, N_EXPERTS, dtype=jnp.float32))
    hid = jax.nn.silu(jnp.einsum('nd,edf->nef', h, w1)) * jnp.einsum('nd,edf->nef', h, w3)
    return jnp.einsum('nef,efd->nd', hid * combine[:, :, None].astype(hid.dtype), w2)


def setup_inputs(seed: int = 0) -> dict:
    key = jax.random.key(seed)
    ks = jax.random.split(key, 40)
    d = D_MODEL
    n_even = (DEPTH + 1) // 2
    n_odd = DEPTH // 2

    def nrm(k, shape, scale):
        return jax.random.normal(k, shape, jnp.float32) * scale

    a_tgt = jax.random.uniform(ks[20], (n_odd, 2, RG_WIDTH), jnp.float32, minval=0.9, maxval=0.999)
    a_root = a_tgt ** (1.0 / RG_C)
    rg_lambda = jnp.log(a_root) - jnp.log1p(-a_root)
    return {
        'x': nrm(ks[0], (BATCH, SEQ, d), 1.0),
        'c': nrm(ks[1], (BATCH, d), 1.0),
        'ctx': nrm(ks[2], (BATCH, CTX_LEN, d), 1.0),
        'c_ctx': nrm(ks[3], (d,), 1.0),
        'ada_w': nrm(ks[4], (DEPTH, d, 6 * d), 0.5 * d ** -0.5),
        'ada_b': nrm(ks[5], (DEPTH, 6 * d), 0.02),
        'norm_mix': 1.0 + nrm(ks[6], (DEPTH, d), 0.02),
        'norm_ffn': 1.0 + nrm(ks[7], (DEPTH, d), 0.02),
        'even_w_in': nrm(ks[8], (n_even, d, EVEN_IN), d ** -0.5),
        'even_w_out': nrm(ks[9], (n_even, MIX_OUT, d), MIX_OUT ** -0.5),
        'hgrn_lb_logits': nrm(ks[10], (2, n_even + 1, A_WIDTH), 0.1),
        'hgrn_out_norm': 1.0 + nrm(ks[11], (n_even, A_DV), 0.02),
        'diff_qk_norm': 1.0 + nrm(ks[12], (n_even, 2, B_DH), 0.02),
        'diff_lambda': nrm(ks[13], (n_even, 4, B_DH), 0.1),
        'diff_out_norm': 1.0 + nrm(ks[14], (n_even, B_DV), 0.02),
        'odd_w_in': nrm(ks[15], (n_odd, d, 2 * RG_WIDTH), d ** -0.5),
        'odd_conv_w': nrm(ks[16], (n_odd, CONV_W, RG_WIDTH), CONV_W ** -0.5),
        'odd_conv_b': nrm(ks[17], (n_odd, RG_WIDTH), 0.01),
        'rg_gate_w': nrm(ks[18], (n_odd, 2, 2, RG_HEADS, RG_BLOCK, RG_BLOCK), RG_BLOCK ** -0.5),
        'rg_gate_b': nrm(ks[19], (n_odd, 2, 2, RG_WIDTH), 0.01),
        'rg_lambda': rg_lambda,
        'odd_w_out': nrm(ks[21], (n_odd, RG_WIDTH, d), RG_WIDTH ** -0.5),
        'moe_w_grp': nrm(ks[22], (DEPTH, d, N_GROUPS), d ** -0.5),
        'moe_b_grp': nrm(ks[23], (DEPTH, N_GROUPS), 0.01),
        'moe_w_exp': nrm(ks[24], (DEPTH, d, N_EXPERTS), d ** -0.5),
        'moe_b_exp': nrm(ks[25], (DEPTH, N_EXPERTS), 0.01),
        'moe_w1': nrm(ks[26], (DEPTH, N_EXPERTS, d, D_EXPERT), d ** -0.5),
        'moe_w3': nrm(ks[27], (DEPTH, N_EXPERTS, d, D_EXPERT), d ** -0.5),
        'moe_w2': nrm(ks[28], (DEPTH, N_EXPERTS, D_EXPERT, d), D_EXPERT ** -0.5),
    }


def reference(x, c, ctx, c_ctx, ada_w, ada_b, norm_mix, norm_ffn,
              even_w_in, even_w_out, hgrn_lb_logits, hgrn_out_norm,
              diff_qk_norm, diff_lambda, diff_out_norm,
              odd_w_in, odd_conv_w, odd_conv_b, rg_gate_w, rg_gate_b, rg_lambda, odd_w_out,
              moe_w_grp, moe_b_grp, moe_w_exp, moe_b_exp, moe_w1, moe_w3, moe_w2):
    bsz, n, d = x.shape
    rope = axial_rope(n)
    lb_all = jnp.cumsum(jax.nn.softmax(hgrn_lb_logits.astype(jnp.float32), axis=1), axis=1)
    for l in range(DEPTH):
        with_ctx = l < DEPTH - 1
        li = l // 2
        m_lat = jnp.split(ada(c, ada_w[l], ada_b[l])[:, None, :], 6, axis=-1)
        m_ctx = jnp.split(ada(c_ctx, ada_w[l], ada_b[l])[None, None, :], 6, axis=-1)
        h_lat = modulate(rms_norm(x, norm_mix[l]), m_lat[0], m_lat[1])
        h_ctx = modulate(rms_norm(ctx, norm_mix[l]), m_ctx[0], m_ctx[1])
        if l % 2 == 0:
            mix_lat, mix_ctx = even_mixer(h_lat, h_ctx, even_w_in[li], even_w_out[li], lb_all[0, li], lb_all[1, li],
                                          hgrn_out_norm[li], diff_qk_norm[li], diff_lambda[li], diff_out_norm[li],
                                          0.8 - 0.6 * math.exp(-0.3 * l), rope, with_ctx)
        else:
            mix_lat, mix_ctx = odd_mixer(h_lat, h_ctx, odd_w_in[li], odd_conv_w[li], odd_conv_b[li],
                                         rg_gate_w[li], rg_gate_b[li], rg_lambda[li], odd_w_out[li], with_ctx)
        x = x + m_lat[2] * mix_lat.astype(x.dtype)
        f_lat = modulate(rms_norm(x, norm_ffn[l]), m_lat[3], m_lat[4]).reshape(-1, d)
        if with_ctx:
            ctx = ctx + m_ctx[2] * mix_ctx.astype(ctx.dtype)
            f_ctx = modulate(rms_norm(ctx, norm_ffn[l]), m_ctx[3], m_ctx[4]).reshape(-1, d)
            y = hier_moe(jnp.concatenate([f_lat, f_ctx], axis=0), moe_w_grp[l], moe_b_grp[l], moe_w_exp[l],
                         moe_b_exp[l], moe_w1[l], moe_w3[l], moe_w2[l])
            ctx = ctx + m_ctx[5] * y[bsz * n:].reshape(ctx.shape).astype(ctx.dtype)
            y = y[:bsz * n]
        else:
            y = hier_moe(f_lat, moe_w_grp[l], moe_b_grp[l], moe_w_exp[l], moe_b_exp[l],
                         moe_w1[l], moe_w3[l], moe_w2[l])
        x = x + m_lat[5] * y.reshape(x.shape).astype(x.dtype)
    return x
```

```python
import math
import numpy as np
import ml_dtypes
import concourse.bass as bass
import concourse.mybir as mybir
from concourse.bass_utils import run_bass_kernel_spmd

F32 = mybir.dt.float32
BF16 = mybir.dt.bfloat16
U8 = mybir.dt.uint8
ALU = mybir.AluOpType
AF = mybir.ActivationFunctionType
AX = mybir.AxisListType

D = 2048
KC = 16
TL = 2048
TCX = 256
T = TL + TCX
NT = T // 128
EPS = 1e-6
NCORES = 8
CH = 64
NCHK = T // CH


class Sched:
    CE = ('pe', 'act', 'dve', 'pool')

    def __init__(self, nc):
        self.nc = nc
        self.eng = {'pe': nc.tensor, 'act': nc.scalar, 'dve': nc.vector, 'pool': nc.gpsimd, 'sp': nc.sync}
        self.ops = []
        self.wr = {}
        self.rd = {}
        self.esem = {e: nc.alloc_semaphore(name='e_' + e) for e in self.CE}
        npool = {'sp': 40, 'pool': 40, 'act': 4}
        self.dsem = {q: [nc.alloc_semaphore(name='d_%s%d' % (q, i)) for i in range(n)] for q, n in npool.items()}

    def _deps(self, i, eng, isdma, r, w):
        deps = []
        for k in r:
            st = self.wr.get(k)
            if st:
                for p in st.values():
                    deps.append((p, 'RAW'))
        for k in w:
            st = self.wr.get(k)
            rs = self.rd.get(k)
            if rs:
                for p in rs.values():
                    deps.append((p, 'WAR'))
            if st:
                for p in st.values():
                    deps.append((p, 'WAW'))
        tag = ('dma', i) if isdma else eng
        for k in r:
            self.rd.setdefault(k, {})[tag] = i
        for k in w:
            if self.rd.get(k):
                self.rd[k] = {}
                self.wr[k] = {tag: i}
            else:
                self.wr.setdefault(k, {})[tag] = i
        return deps

    def op(self, eng, fn, r=(), w=()):
        i = len(self.ops)
        self.ops.append(dict(eng=eng, fn=fn, dma=False, deps=self._deps(i, eng, False, r, w), inc=False))
        return i

    def dma(self, q, fn, r=(), w=()):
        i = len(self.ops)
        self.ops.append(dict(eng=q, fn=fn, dma=True, deps=self._deps(i, q, True, r, w), inc=True))
        return i

    def barrier(self):
        self.ops.append(dict(barrier=True))
        self.wr = {}
        self.rd = {}

    def finalize(self):
        ops = self.ops
        last = {e: None for e in self.CE}
        for i, o in enumerate(ops):
            if o.get('barrier'):
                for e in self.CE:
                    if last[e] is not None:
                        ops[last[e]]['inc'] = True
                continue
            need = []
            for (p, kind) in o['deps']:
                po = ops[p]
                if po['dma']:
                    need.append(p)
                elif po['eng'] == o['eng'] and not o['dma']:
                    if o['eng'] == 'pe' or (kind != 'RAW' and o['eng'] != 'pool'):
                        continue
                    need.append(p)
                else:
                    need.append(p)
            o['need'] = need
            for p in need:
                ops[p]['inc'] = True
            if not o['dma']:
                last[o['eng']] = i
        for e in self.CE:
            if last[e] is not None:
                ops[last[e]]['inc'] = True
        cnt = {e: 0 for e in self.CE}
        duse = {q: 0 for q in self.dsem}
        dval = {}
        dlast = {}
        for i, o in enumerate(ops):
            if o.get('barrier'):
                continue
            if o['dma']:
                q = o['eng']
                pool = self.dsem[q]
                s = pool[duse[q] % len(pool)]
                duse[q] += 1
                key = id(s)
                dval[key] = dval.get(key, 0) + 16
                o['sem'] = s
                o['val'] = dval[key]
                o['prev'] = dlast.get(key)
                dlast[key] = i
            elif o['inc']:
                cnt[o['eng']] += 1
                o['sem'] = self.esem[o['eng']]
                o['val'] = cnt[o['eng']]
        waited = {e: {} for e in self.eng}
        cur = {e: 0 for e in self.CE}
        dcur = {}

        def wait(e, s, v):
            wd = waited[e]
            if wd.get(id(s), 0) >= v:
                return
            wd[id(s)] = v
            self.eng[e].wait_ge(s, v)

        nins = 0
        for i, o in enumerate(ops):
            if o.get('barrier'):
                for e in self.eng:
                    for e2 in self.CE:
                        if cur[e2] > 0:
                            wait(e, self.esem[e2], cur[e2])
                    for key, (s, v) in dcur.items():
                        wait(e, s, v)
                continue
            e = o['eng']
            for p in o['need']:
                po = ops[p]
                wait(e, po['sem'], po['val'])
            if o['dma'] and o['prev'] is not None:
                po = ops[o['prev']]
                wait(e, po['sem'], po['val'])
            ins = o['fn'](self.eng[e])
            nins += 1
            if o['dma']:
                ins.then_inc(o['sem'], 16)
                dcur[id(o['sem'])] = (o['sem'], o['val'])
            elif o['inc']:
                ins.then_inc(o['sem'], 1)
                cur[e] = o['val']
        for e in self.eng:
            for e2 in self.CE:
                if cur[e2] > 0:
                    wait(e, self.esem[e2], cur[e2])
            for key, (s, v) in dcur.items():
                wait(e, s, v)
        return nins


PV_LAYOUT = [
    ('c', 16), ('c_ctx', 16), ('ada_b0', 96), ('ada_b1', 96),
    ('norm_mix0', 16), ('norm_mix1', 16), ('norm_ffn0', 16), ('norm_ffn1', 16),
    ('lbl_f0', 8), ('lbl_f1', 8), ('lbl_b0', 8), ('lbl_b1', 8),
    ('hgrn_gain', 1), ('gq', 1), ('gk', 1), ('dl_a', 1), ('dl_b', 1), ('diff_gain', 1),
    ('conv_w', 64), ('conv_b', 16), ('gate_b', 64), ('rg_lam', 32),
]
PV_OFF = {}
_o = 0
for _n, _k in PV_LAYOUT:
    PV_OFF[_n] = (_o, _k)
    _o += _k
PV_ROWS = ((_o + 127) // 128) * 128
NPG = PV_ROWS // 128


def pack_pvec(inp, b):
    rows = np.zeros((PV_ROWS, 128), np.float32)

    def put(name, arr):
        o, k = PV_OFF[name]
        rows[o:o + k] = np.asarray(arr, np.float32).reshape(k, 128)

    put('c', inp['c'][b])
    put('c_ctx', inp['c_ctx'])
    put('ada_b0', inp['ada_b'][0])
    put('ada_b1', inp['ada_b'][1])
    for l in range(2):
        put('norm_mix%d' % l, inp['norm_mix'][l])
        put('norm_ffn%d' % l, inp['norm_ffn'][l])
    lbl = inp['hgrn_lb_logits']
    put('lbl_f0', lbl[0, 0]); put('lbl_f1', lbl[0, 1]); put('lbl_b0', lbl[1, 0]); put('lbl_b1', lbl[1, 1])
    put('hgrn_gain', inp['hgrn_out_norm'][0])
    put('gq', np.concatenate([inp['diff_qk_norm'][0, 0]] * 2))
    put('gk', np.concatenate([inp['diff_qk_norm'][0, 1]] * 2))
    dl = inp['diff_lambda'][0]
    put('dl_a', np.concatenate([dl[0], dl[2]]))
    put('dl_b', np.concatenate([dl[1], dl[3]]))
    put('diff_gain', inp['diff_out_norm'][0])
    put('conv_w', inp['odd_conv_w'][0])
    put('conv_b', inp['odd_conv_b'][0])
    put('gate_b', inp['rg_gate_b'][0])
    put('rg_lam', inp['rg_lambda'][0])
    return rows


def host_consts():
    c = {}
    c['ident'] = np.eye(128, dtype=np.float32)
    s = np.arange(128)[:, None]
    t = np.arange(128)[None, :]
    same = (s // CH) == (t // CH)
    c['maskf'] = (same & (s <= t)).astype(np.float32)
    c['maskb'] = (same & (s >= t)).astype(np.float32)
    m01 = np.ones((128, T + 1), np.float32)
    m01[:, 0::CH] = 0.0
    c['m01'] = m01
    n = TL
    row = (np.arange(n) // 64).astype(np.float32)
    col = (np.arange(n) % 64).astype(np.float32)
    pairs = 16
    inv = (10000.0 ** (-np.arange(pairs, dtype=np.float32) / pairs)).astype(np.float32)
    ang = np.concatenate([row[:, None] * inv, col[:, None] * inv], axis=-1).astype(np.float32)
    cosv = np.cos(ang).astype(np.float32)
    sinv = np.sin(ang).astype(np.float32)
    pidx = (np.arange(128) % 64) // 2
    c['ropec'] = np.ascontiguousarray(cosv[:, pidx].T)
    c['ropes'] = np.ascontiguousarray(sinv[:, pidx].T)
    rm = np.zeros((128, 128), np.float32)
    for i in range(64):
        rm[2 * i + 1, 2 * i] = -1.0
        rm[2 * i, 2 * i + 1] = 1.0
    c['rotm'] = rm
    bo = np.zeros((128, 128), np.float32)
    bo[:64, :64] = 1.0
    bo[64:, 64:] = 1.0
    c['blk1'] = bo
    hm = np.zeros((128, 2), np.float32)
    hm[:64, 0] = 1.0
    hm[64:, 1] = 1.0
    c['halfm'] = hm
    return c


CONST_SHAPES = {'ident': [128, 128], 'maskf': [128, 128], 'maskb': [128, 128], 'm01': [128, T + 1],
                'ropec': [128, TL], 'ropes': [128, TL], 'rotm': [128, 128], 'blk1': [128, 128], 'halfm': [128, 2]}


class Prog:
    def __init__(self, debug=False, upto=99):
        self.debug = debug
        self.upto = upto
        nc = bass.Bass("TRN2", target_bir_lowering=False)
        self.nc = nc
        self.S = Sched(nc)
        self.live = []
        self._uid = 0
        kin = "ExternalInput"
        dt = lambda name, shape, dtype, kind: nc.dram_tensor(name, shape, dtype, kind=kind).ap()
        self.xin = dt("xin", [T, D], F32, kin)
        self.pvec = dt("pvec", [PV_ROWS, 128], F32, kin)
        self.cst = {k: dt("c_" + k, shp, F32, kin) for k, shp in CONST_SHAPES.items()}
        self.ada_w = dt("ada_w", [2, D, 6 * D], F32, kin)
        self.w_in0 = dt("w_in0", [D, 8192], F32, kin)
        self.w_out0 = dt("w_out0", [D, D], F32, kin)
        self.w_in1 = dt("w_in1", [D, 4096], F32, kin)
        self.w_out1 = dt("w_out1", [D, D], F32, kin)
        self.gate_w = dt("gate_w", [2, 2, 16, 128, 128], F32, kin)
        self.moe_wr = dt("moe_wr", [2, D, 20], F32, kin)
        self.moe_br = dt("moe_br", [2, 20], F32, kin)
        self.moe_w1 = dt("moe_w1", [2, 16, D, 512], F32, kin)
        self.moe_w3 = dt("moe_w3", [2, 16, D, 512], F32, kin)
        self.moe_w2 = dt("moe_w2", [2, 16, 512, D], F32, kin)
        self.out = dt("out", [TL, D], F32, "ExternalOutput")
        sk = "ExternalOutput" if debug else "Internal"
        self.xs = dt("xs", [T, D], F32, sk)
        self.projT = dt("projT", [8192, T], F32, sk)
        self.vtok = dt("vtok", [16, T, 128], BF16, sk)
        self.mixT = dt("mixT", [D, T], BF16, sk)
        self.w1b = dt("w1b", [2, 16, D, 512], BF16, "Internal")
        self.w3b = dt("w3b", [2, 16, D, 512], BF16, "Internal")
        self.w2b = dt("w2b", [2, 16, 512, D], BF16, "Internal")
        if debug:
            self.dbg = dt("dbg", [128, 2048], F32, "ExternalOutput")
        self.ps = [nc.alloc_psum_tensor("ps%d" % i, [128, 512], F32) for i in range(8)]
        self.psi = 0

    def sb(self, name, shape, dtype):
        g = self.nc.sbuf_tensor(name + "_%d" % self._uid, shape, dtype)
        t = g.__enter__()
        self._uid += 1
        self.live.append(g)
        return t

    def free_phase(self, keep=0):
        self.S.barrier()
        while len(self.live) > keep:
            self.live.pop().__exit__(None, None, None)

    def bank(self, exclude=(), only=None):
        if only is None:
            exclude = tuple(exclude) + tuple(getattr(self, 'bank_reserved', ()))
        while True:
            i = self.psi
            self.psi = (self.psi + 1) % 8
            if i not in exclude and (only is None or i in only):
                return i

    def load_consts(self):
        S = self.S
        self.ident = self.sb("ident", [128, 128], F32)
        S.dma('sp', lambda e: e.dma_start(out=self.ident[:], in_=self.cst['ident'][:, :]), w=['ident'])
        self.ones = self.sb("ones", [128, 128], F32)
        S.op('dve', lambda e: e.memset(self.ones[:], 1.0), w=['ones'])
        self.onesb = self.sb("onesb", [128, 128], BF16)
        S.op('dve', lambda e: e.memset(self.onesb[:], 1.0), w=['onesb'])
        self.identb = self.sb("identb", [128, 128], BF16)
        S.op('dve', lambda e: e.tensor_copy(out=self.identb[:], in_=self.ident[:]), r=['ident'], w=['identb'])

    def load_params(self):
        S = self.S
        for g in range(NPG):
            raw = self.sb("praw%d" % g, [128, 128], F32)
            S.dma('sp', lambda e, raw=raw, g=g: e.dma_start(out=raw[:], in_=self.pvec[g * 128:(g + 1) * 128, :]),
                  w=[('praw', g)])
            b = self.bank()
            S.op('pe', lambda e, raw=raw, b=b: e.transpose(out=self.ps[b][:, 0:128], in_=raw[:], identity=self.ident[:]),
                 r=[('praw', g), 'ident'], w=[('ps', b)])
            S.op('dve', lambda e, b=b, g=g: e.tensor_copy(out=self.PC[:, g * 128:(g + 1) * 128], in_=self.ps[b][:, 0:128]),
                 r=[('ps', b)], w=['PC'])

    def pc(self, name, i=0, n=None):
        o, k = PV_OFF[name]
        if n is None:
            n = k - i
        return self.PC[:, o + i:o + i + n]

    def ada_begin(self, units):
        self._ada_ring = [self.sb("adaw%d" % i, [128, KC, 128], F32) for i in range(3)]
        self._ada_ringb = [self.sb("adawb%d" % i, [128, KC, 128], BF16) for i in range(2)]
        if not hasattr(self, '_ada_sc'):
            raise RuntimeError
        self._ada_q = list(units)
        self._ada_cast_eng = 'pool'
        self._ada_dma_i = 0
        self._ada_mm_i = 0
        self._ada_base = getattr(self, '_ada_base', 0)

    def ada_step(self, n=1):
        S = self.S
        sc = self._ada_sc
        for _ in range(n):
            while self._ada_dma_i < len(self._ada_q) and self._ada_dma_i < self._ada_mm_i + 3:
                l, fj = self._ada_q[self._ada_dma_i]
                slot = (self._ada_base + self._ada_dma_i) % 3
                wt = self._ada_ring[self._ada_dma_i % 3]
                S.dma('sp', lambda e, wt=wt, l=l, fj=fj: e.dma_start(
                    out=wt[:], in_=self.ada_w[l, :, fj * 128:(fj + 1) * 128].rearrange("(k p) f -> p k f", p=128)),
                    w=[('adaw', self._ada_dma_i % 3)])
                self._ada_dma_i += 1
            if self._ada_mm_i >= len(self._ada_q):
                return
            l, fj = self._ada_q[self._ada_mm_i]
            wt = self._ada_ring[self._ada_mm_i % 3]
            wk = ('adaw', self._ada_mm_i % 3)
            wb = self._ada_ringb[self._ada_mm_i % 2]
            wbk = ('adawb', self._ada_mm_i % 2)
            self._ada_mm_i += 1
            if self._ada_mm_i % 3 == 0:
                S.op('act', lambda e, wt=wt, wb=wb: e.activation(out=wb[:], in_=wt[:], func=AF.Copy), r=[wk], w=[wbk])
            else:
                S.op('dve', lambda e, wt=wt, wb=wb: e.tensor_copy(out=wb[:], in_=wt[:]), r=[wk], w=[wbk])
            b = self.bank(only=self._ada_banks) if getattr(self, '_ada_banks', None) else self.bank()
            for kc in range(KC):
                S.op('pe', lambda e, wb=wb, kc=kc, b=b: e.matmul(self.ps[b][:, 0:2], lhsT=wb[:, kc, :], rhs=self._ada_scb[:, kc, :],
                                                               start=(kc == 0), stop=(kc == KC - 1)), r=[wbk, 'scb'], w=[('ps', b)])
            o, k = PV_OFF['ada_b%d' % l]
            S.op('dve', lambda e, l=l, fj=fj, b=b, o=o: e.tensor_tensor(
                out=self.M[l][:, fj, :], in0=self.ps[b][:, 0:2], in1=self.PC[:, o + fj:o + fj + 1].to_broadcast([128, 2]), op=ALU.add),
                r=[('ps', b), 'PC'], w=[('M', l, fj // KC)])

    def ada_flush(self):
        while self._ada_mm_i < len(self._ada_q):
            self.ada_step(1)

    def mcol(self, l, which, kc, ctx):
        return self.M[l][:, which * KC + kc, (1 if ctx else 0):(2 if ctx else 1)]

    def make_gs(self, l, nrm):
        S = self.S
        if not hasattr(self, 'GS'):
            self.GS = {}
        for ctx in range(2):
            g = self.GSbuf[(l, nrm, ctx)]
            sidx = 1 if nrm == 0 else 4
            gname = ('norm_mix%d' if nrm == 0 else 'norm_ffn%d') % l
            S.op('dve', lambda e, g=g, l=l, sidx=sidx, ctx=ctx, gname=gname: e.scalar_tensor_tensor(
                out=g[:], in0=self.M[l][:, sidx * KC:(sidx + 1) * KC, ctx], scalar=1.0, in1=self.pc(gname),
                op0=ALU.add, op1=ALU.mult), r=[('M', l, sidx), 'PC'], w=[('GS', l, nrm, ctx)])
            self.GS[(l, nrm, ctx)] = g

    def nmt(self, src, tiles, l, nrm, hT, h32=None, tag=""):
        S = self.S
        shidx = 0 if nrm == 0 else 3
        if not hasattr(self, '_nmt_bufs'):
            self._nmt_bufs = None
        xts = [self.sb("nmt_x%d" % i, [128, D], F32) for i in range(2)]
        junk = self.sb("nmt_junk", [128, D], BF16)
        st = self.sb("nmt_st", [128, 4 * NT], F32)
        for n, i in enumerate(tiles):
            ctx = 1 if i >= TL // 128 else 0
            xt = xts[n % 2]
            xk = ('nmt_x', n % 2)
            S.dma('sp', lambda e, xt=xt, i=i: e.dma_start(out=xt[:], in_=src[i * 128:(i + 1) * 128, :]),
                  r=[('xs', i)], w=[xk])
            ss = st[:, 4 * n:4 * n + 1]
            rs = st[:, 4 * n + 1:4 * n + 2]
            sk = ('nmt_st', n)
            S.op('act', lambda e, xt=xt, ss=ss: e.activation(out=junk[:], in_=xt[:], func=AF.Square, accum_out=ss),
                 r=[xk], w=[sk, 'nmt_junk'])
            S.op('act', lambda e, ss=ss: e.activation(out=ss, in_=ss, func=AF.Sqrt, bias=self.epsc[:, 0:1], scale=1.0 / D),
                 r=[sk, 'epsc'], w=[sk])
            S.op('dve', lambda e, ss=ss, rs=rs: e.reciprocal(out=rs, in_=ss), r=[sk], w=[sk])
            S.op('dve', lambda e, xt=xt, rs=rs: e.tensor_scalar(out=xt[:], in0=xt[:], scalar1=rs, scalar2=None, op0=ALU.mult),
                 r=[sk, xk], w=[xk])
            gs = self.GS[(l, nrm, ctx)]
            for q in range(4):
                b = self.bank()
                for j in range(4):
                    kc = q * 4 + j
                    S.op('pe', lambda e, xt=xt, b=b, j=j, kc=kc: e.transpose(
                        out=self.ps[b][:, j * 128:(j + 1) * 128], in_=xt[:, kc * 128:(kc + 1) * 128], identity=self.ident[:]),
                        r=[xk, 'ident'], w=[('ps', b)])
                for j in range(4):
                    kc = q * 4 + j
                    dst = hT[:, kc, i * 128:(i + 1) * 128] if h32 is None else h32[:, kc, :]
                    wk = ('hT', tag, i) if h32 is None else ('h32', tag)
                    eng = 'act' if (j % 2 == 0) else 'dve'
                    sh = self.mcol(l, shidx, kc, ctx)
                    if eng == 'act':
                        S.op('act', lambda e, dst=dst, b=b, j=j, kc=kc, gs=gs, sh=sh: e.activation(
                            out=dst, in_=self.ps[b][:, j * 128:(j + 1) * 128], func=AF.Identity, bias=sh, scale=gs[:, kc:kc + 1]),
                            r=[('ps', b), ('GS', l, nrm, ctx), ('M', l, shidx)], w=[wk])
                    else:
                        S.op('dve', lambda e, dst=dst, b=b, j=j, kc=kc, gs=gs, sh=sh: e.tensor_scalar(
                            out=dst, in0=self.ps[b][:, j * 128:(j + 1) * 128], scalar1=gs[:, kc:kc + 1], scalar2=sh,
                            op0=ALU.mult, op1=ALU.add),
                            r=[('ps', b), ('GS', l, nrm, ctx), ('M', l, shidx)], w=[wk])
            if h32 is not None:
                S.op('pool', lambda e, i=i: e.tensor_copy(out=hT[:, :, i * 128:(i + 1) * 128], in_=h32[:, :, :]),
                     r=[('h32', tag)], w=[('hT', tag, i)])

    def tblocks(self, t0, t1):
        out = []
        t = t0
        while t < t1:
            n = min(512, t1 - t)
            out.append((t, n))
            t += n
        return out

    def linear_fm(self, w, groups, hT, tag, evac, done, t1=T):
        S = self.S
        slabs = [self.sb("wslab%d" % i, [128, KC, 512], BF16) for i in range(2)]
        for gi, grp in enumerate(groups):
            sl = slabs[gi % 2]
            sk = ('wslab', gi % 2)
            c0 = grp[0] * 128
            ncol = len(grp) * 128
            for half in range(2):
                S.dma('pool', lambda e, sl=sl, c0=c0, ncol=ncol, half=half: e.dma_start(
                    out=sl[:, half * 8:(half + 1) * 8, 0:ncol],
                    in_=w[half * 1024:(half + 1) * 1024, c0:c0 + ncol].rearrange("(k p) f -> p k f", p=128)), w=[sk])
            for jj, g in enumerate(grp):
                for (t0, n) in self.tblocks(0, t1):
                    b = self.bank()
                    for kc in range(KC):
                        S.op('pe', lambda e, sl=sl, jj=jj, kc=kc, b=b, t0=t0, n=n: e.matmul(
                            self.ps[b][:, 0:n], lhsT=sl[:, kc, jj * 128:(jj + 1) * 128], rhs=hT[:, kc, t0:t0 + n],
                            start=(kc == 0), stop=(kc == KC - 1)),
                            r=[sk] + [('hT', tag, i) for i in range(t0 // 128, (t0 + n) // 128)], w=[('ps', b)])
                    evac(g, t0, n, self.ps[b][:, 0:n], b)
                done(g)

    def linear_tm(self, w, colblocks, hT, tag, tiles, evac):
        S = self.S
        slabs = [self.sb("wslabt%d" % i, [128, KC, 512], BF16) for i in range(2)]
        for gi, c0 in enumerate(colblocks):
            sl = slabs[gi % 2]
            sk = ('wslabt', gi % 2)
            for half in range(2):
                S.dma('pool', lambda e, sl=sl, c0=c0, half=half: e.dma_start(
                    out=sl[:, half * 8:(half + 1) * 8, :],
                    in_=w[half * 1024:(half + 1) * 1024, c0:c0 + 512].rearrange("(k p) f -> p k f", p=128)), w=[sk])
            for i in tiles:
                b = self.bank()
                for kc in range(KC):
                    S.op('pe', lambda e, sl=sl, kc=kc, b=b, i=i: e.matmul(
                        self.ps[b][:, :], lhsT=hT[:, kc, i * 128:(i + 1) * 128], rhs=sl[:, kc, :],
                        start=(kc == 0), stop=(kc == KC - 1)),
                        r=[sk, ('hT', tag, i)], w=[('ps', b)])
                evac(gi, c0, i, self.ps[b][:, :], b)

    def phase_prep(self):
        S = self.S
        self.load_consts()
        self.epsc = self.sb("epsc", [128, 1], F32)
        S.op('dve', lambda e: e.memset(self.epsc[:], EPS), w=['epsc'])
        self.PC = self.sb("PC", [128, PV_ROWS], F32)
        self.M = [self.sb("M%d" % l, [128, 96, 2], F32) for l in range(2)]
        self.GSbuf = {(l, nrm, ctx): self.sb("gs%d%d%d" % (l, nrm, ctx), [128, KC], F32)
                      for l in range(2) for nrm in range(2) for ctx in range(2)}
        self.LB = self.sb("LB", [128, 16], F32)
        self.OML = self.sb("OML", [128, 16], F32)
        self.NEGLAM = self.sb("NEGLAM", [128, 1], F32)
        self._ada_sc = self.sb("silu_c", [128, KC, 2], F32)
        self._ada_scb = self.sb("silu_cb", [128, KC, 2], BF16)
        self.nkeep = len(self.live)
        self.load_params()
        S.op('act', lambda e: e.activation(out=self._ada_sc[:, :, 0], in_=self.pc('c'), func=AF.Silu), r=['PC'], w=['sc'])
        S.op('act', lambda e: e.activation(out=self._ada_sc[:, :, 1], in_=self.pc('c_ctx'), func=AF.Silu), r=['PC'], w=['sc'])
        S.op('dve', lambda e: e.tensor_copy(out=self._ada_scb[:], in_=self._ada_sc[:]), r=['sc'], w=['scb'])
        self.make_lam(0.8 - 0.6 * math.exp(-0.3 * 0))
        for d_, (n0, n1) in enumerate((('lbl_f0', 'lbl_f1'), ('lbl_b0', 'lbl_b1'))):
            S.op('dve', lambda e, d_=d_, n0=n0, n1=n1: e.tensor_tensor(out=self.LB[:, d_ * 8:(d_ + 1) * 8], in0=self.pc(n0),
                                                                  in1=self.pc(n1), op=ALU.subtract), r=['PC'], w=['LB'])
        S.op('act', lambda e: e.activation(out=self.LB[:], in_=self.LB[:], func=AF.Sigmoid), r=['LB'], w=['LB'])
        S.op('dve', lambda e: e.tensor_scalar(out=self.OML[:], in0=self.LB[:], scalar1=-1.0, scalar2=1.0, op0=ALU.mult, op1=ALU.add),
             r=['LB'], w=['OML'])
        self.ada_begin([(0, fj) for fj in range(2 * KC)])
        self.ada_flush()
        self.make_gs(0, 0)
        self.free_phase(self.nkeep)

    def phase_inproj0(self):
        S = self.S
        hT = self.sb("hT", [128, KC, T], BF16)
        self.nmt(self.xin, list(range(NT)), 0, 0, hT, tag="a")
        stg = [self.sb("stg%d" % i, [128, T], F32) for i in range(2)]
        cnt = [0]

        def evac(g, t0, n, pap, b):
            st = stg[cnt[0] % 2]
            fam = g // 8
            func = AF.Silu if fam in (0, 4) else AF.Copy
            S.op('act', lambda e, st=st, t0=t0, n=n, pap=pap, func=func: e.activation(out=st[:, t0:t0 + n], in_=pap, func=func),
                 r=[('ps', b)], w=[('stg', cnt[0] % 2)])

        def done(g):
            st = stg[cnt[0] % 2]
            S.dma('sp', lambda e, st=st, g=g: e.dma_start(out=self.projT[g * 128:(g + 1) * 128, :], in_=st[:]),
                  r=[('stg', cnt[0] % 2)], w=[('projT', g)])
            cnt[0] += 1

        fm_groups = []
        for fam in (0, 1, 2, 4, 5, 6):
            fm_groups.append([fam * 8 + j for j in range(4)])
            fm_groups.append([fam * 8 + 4 + j for j in range(4)])
        self.linear_fm(self.w_in0, fm_groups, hT, "a", evac, done)
        vst = [self.sb("vst%d" % i, [128, 512], BF16) for i in range(2)]
        vc = [0]

        def evac_v(gi, c0, i, pap, b):
            st = vst[vc[0] % 2]
            k = ('vst', vc[0] % 2)
            h0 = gi * 4
            S.op('act', lambda e, st=st, pap=pap: e.activation(out=st[:], in_=pap, func=AF.Copy), r=[('ps', b)], w=[k])
            S.dma('sp', lambda e, st=st, h0=h0, i=i: e.dma_start(
                out=self.vtok[h0:h0 + 4, i * 128:(i + 1) * 128, :].rearrange("h t c -> t h c"),
                in_=st[:].rearrange("t (h c) -> t h c", c=128)), r=[k], w=[('vtok', h0, i)])
            vc[0] += 1

        self.linear_tm(self.w_in0, [3072, 3584, 7168, 7680], hT, "a", list(range(NT)), evac_v)
        self.free_phase(self.nkeep)


    def rms_gate_store(self, oT, gate_col, post_scale, sg, row0, tcols, pfx):
        S = self.S
        sq = self.sb(pfx + "sq", [128, T], BF16)
        rstd = self.sb(pfx + "rstd", [128, T], F32)
        ob = self.sb(pfx + "ob", [128, T], BF16)
        S.op('act', lambda e: e.activation(out=sq[:, 0:tcols], in_=oT[:, 0:tcols], func=AF.Square), r=[pfx + 'oT'], w=[pfx + 'sq'])
        for (t0, n) in self.tblocks(0, tcols):
            b = self.bank()
            S.op('pe', lambda e, b=b, t0=t0, n=n: e.matmul(self.ps[b][:, 0:n], lhsT=self.onesb[:], rhs=sq[:, t0:t0 + n],
                                                          start=True, stop=True), r=[pfx + 'sq', 'onesb'], w=[('ps', b)])
            S.op('act', lambda e, b=b, t0=t0, n=n: e.activation(out=rstd[:, t0:t0 + n], in_=self.ps[b][:, 0:n], func=AF.Sqrt,
                                                               bias=self.epsc[:, 0:1], scale=1.0 / 128.0),
                 r=[('ps', b), 'epsc'], w=[pfx + 'rstd'])
        S.op('dve', lambda e: e.reciprocal(out=rstd[:, 0:tcols], in_=rstd[:, 0:tcols]), r=[pfx + 'rstd'], w=[pfx + 'rstd'])
        S.op('dve', lambda e: e.tensor_tensor(out=rstd[:, 0:tcols], in0=rstd[:, 0:tcols], in1=oT[:, 0:tcols], op=ALU.mult),
             r=[pfx + 'rstd', pfx + 'oT'], w=[pfx + 'rstd'])
        if sg is not None:
            S.op('dve', lambda e: e.scalar_tensor_tensor(out=ob[:, 0:tcols], in0=rstd[:, 0:tcols], scalar=gate_col, in1=sg[:, 0:tcols],
                                                        op0=ALU.mult, op1=ALU.mult), r=[pfx + 'rstd', pfx + 'sg', 'PC'], w=[pfx + 'ob'])
        else:
            S.op('dve', lambda e: e.tensor_scalar(out=ob[:, 0:tcols], in0=rstd[:, 0:tcols], scalar1=gate_col, scalar2=post_scale,
                                                 op0=ALU.mult, op1=ALU.mult), r=[pfx + 'rstd', 'PC'], w=[pfx + 'ob'])
        return ob

    def phase_hgrn(self):
        S = self.S
        self.ada_begin([(0, fj) for fj in range(2 * KC, 6 * KC)] + [(1, fj) for fj in range(6 * KC)])
        m01 = self.sb("m01", [128, T + 1], F32)
        S.dma('sp', lambda e: e.dma_start(out=m01[:], in_=self.cst['m01'][:, :]), w=['m01'])
        masks = []
        for nm in ('maskf', 'maskb'):
            mk = self.sb(nm, [128, 128], F32)
            S.dma('sp', lambda e, mk=mk, nm=nm: e.dma_start(out=mk[:], in_=self.cst[nm][:, :]), w=[nm])
            masks.append(mk)
        qT = self.sb("hq", [128, T], F32)
        fl = [self.sb("hf%d" % d_, [128, T], F32) for d_ in range(2)]
        sgs = [self.sb("hg%d" % i, [128, T], F32) for i in range(2)]
        vtms = [self.sb("hv%d" % i, [128, NT, 128], BF16) for i in range(2)]
        kT = self.sb("hk", [128, T], F32)
        cum = self.sb("hcum", [128, T], F32)
        E = self.sb("hE", [128, T], F32)
        k2f = self.sb("hk2f", [128, T], F32)
        oT = k2f
        qt = [self.sb("hqt%d" % d_, [128, T], BF16) for d_ in range(2)]
        kt = [self.sb("hkt%d" % d_, [128, T], BF16) for d_ in range(2)]
        k2t = [self.sb("hk2t%d" % d_, [128, NT, 128], BF16) for d_ in range(2)]
        Sbf = [self.sb("hSbf%d" % d_, [128, NCHK, 128], BF16) for d_ in range(2)]
        dec = [self.sb("hdec%d" % d_, [128, NCHK], F32) for d_ in range(2)]
        Sst = [[self.sb("hS%d_%d" % (d_, i), [128, 128], F32) for i in range(2)] for d_ in range(2)]
        attm = [self.sb("hattm%d" % i, [128, 128], BF16) for i in range(4)]
        nlat = TL // CH

        def load_qf(h):
            S.dma('sp', lambda e: e.dma_start(out=qT[:], in_=self.projT[h * 128:(h + 1) * 128, :]), r=[('projT', h)], w=['hq'])
            for d_ in range(2):
                S.dma('sp', lambda e, d_=d_: e.dma_start(out=fl[d_][:], in_=self.projT[(8 + d_ * 8 + h) * 128:(9 + d_ * 8 + h) * 128, :]),
                      r=[('projT', 8 + d_ * 8 + h)], w=[('hf', d_)])

        def load_gv(h):
            S.dma('sp', lambda e: e.dma_start(out=sgs[h % 2][:], in_=self.projT[(32 + h) * 128:(33 + h) * 128, :]), r=[('projT', 32 + h)], w=[('hsg', h % 2)])
            S.dma('sp', lambda e: e.dma_start(out=vtms[h % 2][:], in_=self.vtok[h, :, :].rearrange("(n p) c -> p n c", p=128)),
                  r=[('vtok', (h // 4) * 4, i) for i in range(NT)], w=[('hv', h % 2)])

        load_qf(0)
        load_gv(0)
        for h in range(8):
            sg = sgs[h % 2]
            vtm = vtms[h % 2]
            vk = ('hv', h % 2)
            if h + 1 < 8:
                load_gv(h + 1)
            for d_ in range(2):
                f = fl[d_]
                fk = ('hf', d_)
                lbc = self.LB[:, d_ * 8 + h:d_ * 8 + h + 1]
                omc = self.OML[:, d_ * 8 + h:d_ * 8 + h + 1]
                S.op('act', lambda e, f=f: e.activation(out=f[:], in_=f[:], func=AF.Sigmoid), r=[fk], w=[fk])
                S.op('dve', lambda e, f=f, lbc=lbc, omc=omc: e.tensor_scalar(out=f[:], in0=f[:], scalar1=omc, scalar2=lbc,
                                                                           op0=ALU.mult, op1=ALU.add), r=[fk, 'LB', 'OML'], w=[fk])
                S.op('pool', lambda e, f=f: e.tensor_scalar(out=kT[:], in0=f[:], scalar1=-1.0, scalar2=1.0, op0=ALU.mult, op1=ALU.add),
                     r=[fk], w=['hk'])
                S.op('act', lambda e, f=f: e.activation(out=f[:], in_=f[:], func=AF.Ln), r=[fk, 'hk'], w=[fk])
                if d_ == 0:
                    S.op('dve', lambda e, f=f: e.tensor_tensor_scan(out=cum[:, 0:T], data0=m01[:, 0:T], data1=f[:, 0:T], initial=0.0,
                                                                   op0=ALU.mult, op1=ALU.add), r=[fk, 'm01'], w=['hcum'])
                else:
                    S.op('dve', lambda e, f=f: e.tensor_tensor_scan(out=cum[:, ::-1], data0=m01[:, 1:T + 1][:, ::-1], data1=f[:, ::-1],
                                                                   initial=0.0, op0=ALU.mult, op1=ALU.add), r=[fk, 'm01'], w=['hcum'])
                S.op('act', lambda e: e.activation(out=E[:], in_=cum[:], func=AF.Exp), r=['hcum'], w=['hE'])
                S.op('dve', lambda e, d_=d_: e.tensor_tensor(out=qt[d_][:], in0=qT[:], in1=E[:], op=ALU.mult), r=['hq', 'hE'], w=[('hqt', d_)])
                S.op('act', lambda e: e.activation(out=E[:], in_=cum[:], func=AF.Exp, scale=-1.0), r=['hcum', ('hqt', d_)], w=['hE'])
                S.op('dve', lambda e, d_=d_: e.tensor_tensor(out=kt[d_][:], in0=kT[:], in1=E[:], op=ALU.mult), r=['hk', 'hE'], w=[('hkt', d_)])
                cum3 = cum[:].rearrange("p (c j) -> p c j", j=CH)
                lastj = (CH - 1) if d_ == 0 else 0
                S.op('act', lambda e, d_=d_, cum3=cum3, lastj=lastj: e.activation(out=dec[d_][:], in_=cum3[:, :, lastj], func=AF.Exp),
                     r=['hcum'], w=[('hdec', d_)])
                E3 = E[:].rearrange("p (c j) -> p c j", j=CH)
                S.op('dve', lambda e, cum3=cum3, E3=E3, lastj=lastj: e.tensor_tensor(
                    out=E3, in0=cum3[:, :, lastj:lastj + 1].to_broadcast([128, NCHK, CH]), in1=cum3, op=ALU.subtract),
                    r=['hcum', ('hkt', d_)], w=['hE'])
                S.op('act', lambda e: e.activation(out=E[:], in_=E[:], func=AF.Exp), r=['hE'], w=['hE'])
                S.op('dve', lambda e: e.tensor_tensor(out=k2f[:], in0=kT[:], in1=E[:], op=ALU.mult), r=['hk', 'hE'], w=['hk2f'])
                for q in range((NT + 3) // 4):
                    b = self.bank()
                    nt_ = min(4, NT - q * 4)
                    for j in range(nt_):
                        i = q * 4 + j
                        S.op('pe', lambda e, b=b, j=j, i=i: e.transpose(out=self.ps[b][:, j * 128:(j + 1) * 128],
                                                                        in_=k2f[:, i * 128:(i + 1) * 128], identity=self.ident[:]),
                             r=['hk2f', 'ident'], w=[('ps', b)])
                    S.op('act', lambda e, b=b, q=q, nt_=nt_, d_=d_: e.activation(
                        out=k2t[d_][:, q * 4:q * 4 + nt_, :], in_=self.ps[b][:, 0:nt_ * 128].rearrange("p (n c) -> p n c", c=128),
                        func=AF.Copy), r=[('ps', b)], w=[('hk2t', d_)])
            if h + 1 < 8:
                load_qf(h + 1)
            orders = [list(range(nlat, NCHK)) + list(range(0, nlat)),
                      list(range(NCHK - 1, nlat - 1, -1)) + list(range(nlat - 1, -1, -1))]
            cb_ = [None, None]
            for n in range(NCHK):
                if n % 2 == 0:
                    self.ada_step(1)
                for d_ in range(2):
                    c = orders[d_][n]
                    if n % 4 == 0:
                        cb_[d_] = self.bank()
                    b = cb_[d_]
                    i, half = c // 2, c % 2
                    pa = self.ps[b][:, (n % 4) * 128:(n % 4 + 1) * 128]
                    S.op('pe', lambda e, pa=pa, i=i, half=half, d_=d_, vtm=vtm: e.matmul(
                        pa, lhsT=k2t[d_][half * 64:(half + 1) * 64, i, :], rhs=vtm[half * 64:(half + 1) * 64, i, :], start=True, stop=True),
                        r=[('hk2t', d_), vk], w=[('ps', b)])
                    Sn = Sst[d_][n % 2]
                    So = Sst[d_][(n + 1) % 2]
                    if n == 0:
                        S.op('dve', lambda e, Sn=Sn, pa=pa: e.tensor_copy(out=Sn[:], in_=pa), r=[('ps', b)], w=[('hS', d_, n % 2)])
                    else:
                        S.op('dve', lambda e, Sn=Sn, So=So, pa=pa, c=c, d_=d_: e.scalar_tensor_tensor(
                            out=Sn[:], in0=So[:], scalar=dec[d_][:, c:c + 1], in1=pa, op0=ALU.mult, op1=ALU.add),
                            r=[('ps', b), ('hS', d_, (n + 1) % 2), ('hdec', d_)], w=[('hS', d_, n % 2)])
                    if n + 1 < NCHK:
                        cn = orders[d_][n + 1]
                        S.op('act', lambda e, Sn=Sn, cn=cn, d_=d_: e.activation(out=Sbf[d_][:, cn, :], in_=Sn[:], func=AF.Copy),
                             r=[('hS', d_, n % 2)], w=[('hSbf', d_, cn)])
            first = [nlat, NCHK - 1]

            def emit_att(i):
                for d_ in range(2):
                    ba = 4 + ((2 * i + d_) % 4)
                    am = attm[(2 * i + d_) % 4]
                    S.op('pe', lambda e, ba=ba, i=i, d_=d_: e.matmul(self.ps[ba][:, 0:128], lhsT=kt[d_][:, i * 128:(i + 1) * 128],
                                                                     rhs=qt[d_][:, i * 128:(i + 1) * 128], start=True, stop=True),
                         r=[('hkt', d_), ('hqt', d_)], w=[('ps', ba)])
                    S.op('dve', lambda e, ba=ba, am=am, d_=d_: e.tensor_tensor(out=am[:], in0=self.ps[ba][:, 0:128], in1=masks[d_][:], op=ALU.mult),
                         r=[('ps', ba), 'maskf', 'maskb'], w=[('hattm', (2 * i + d_) % 4)])

            emit_att(0)
            for i in range(NT):
                if i + 1 < NT:
                    emit_att(i + 1)
                q, j = i // 4, i % 4
                bo = q % 4
                po = self.ps[bo][:, j * 128:(j + 1) * 128]
                mms = []
                for d_ in range(2):
                    mms.append(('pv', d_, None, None))
                    for half in range(2):
                        c = 2 * i + half
                        if c != first[d_]:
                            mms.append(('s', d_, c, half))
                for mi, (kind, d_, c, half) in enumerate(mms):
                    st_, sp_ = (mi == 0), (mi == len(mms) - 1)
                    if kind == 'pv':
                        am = attm[(2 * i + d_) % 4]
                        S.op('pe', lambda e, po=po, i=i, am=am, st_=st_, sp_=sp_, vtm=vtm: e.matmul(po, lhsT=vtm[:, i, :], rhs=am[:], start=st_, stop=sp_),
                             r=[vk, ('hattm', (2 * i + d_) % 4)], w=[('ps', bo)])
                    else:
                        S.op('pe', lambda e, po=po, c=c, half=half, d_=d_, st_=st_, sp_=sp_: e.matmul(
                            po[:, half * 64:(half + 1) * 64], lhsT=Sbf[d_][:, c, :], rhs=qt[d_][:, c * 64:(c + 1) * 64], start=st_, stop=sp_),
                            r=[('hSbf', d_, c), ('hqt', d_)], w=[('ps', bo)])
                if j == 3 or i == NT - 1:
                    nt_ = j + 1
                    S.op('act', lambda e, bo=bo, q=q, nt_=nt_: e.activation(out=oT[:, q * 512:q * 512 + nt_ * 128], in_=self.ps[bo][:, 0:nt_ * 128], func=AF.Copy),
                         r=[('ps', bo)], w=['hk2f'])
            self.ada_step(2)
            ob = self._hg_out(oT, sg, h, okey='hk2f', sgkey=('hsg', h % 2))
        self.ada_flush()
        self.make_gs(0, 1)
        self.make_gs(1, 0)
        self.make_gs(1, 1)
        self.free_phase(self.nkeep)

    def make_lam(self, lam_init):
        S = self.S
        hm = self.sb("halfm", [128, 2], F32)
        S.dma('sp', lambda e: e.dma_start(out=hm[:], in_=self.cst['halfm'][:, :]), w=['halfm'])
        pr = self.sb("lamp", [128, 4], F32)
        S.op('dve', lambda e: e.tensor_tensor(out=pr[:, 0:1], in0=self.pc('dl_a'), in1=self.pc('dl_b'), op=ALU.mult), r=['PC'], w=['lamp'])
        S.op('dve', lambda e: e.tensor_scalar(out=pr[:, 2:4], in0=hm[:], scalar1=pr[:, 0:1], scalar2=None, op0=ALU.mult), r=['lamp', 'halfm'], w=['lamp2'])
        b = self.bank()
        S.op('pe', lambda e: e.matmul(self.ps[b][:, 0:2], lhsT=self.ones[:], rhs=pr[:, 2:4], start=True, stop=True), r=['lamp2', 'ones'], w=[('ps', b)])
        S.op('act', lambda e: e.activation(out=pr[:, 2:4], in_=self.ps[b][:, 0:2], func=AF.Exp), r=[('ps', b)], w=['lamp2'])
        S.op('dve', lambda e: e.tensor_tensor(out=pr[:, 0:1], in0=pr[:, 3:4], in1=pr[:, 2:3], op=ALU.subtract), r=['lamp2'], w=['lamp'])
        S.op('dve', lambda e: e.tensor_scalar(out=self.NEGLAM[:], in0=pr[:, 0:1], scalar1=-lam_init, scalar2=None, op0=ALU.add), r=['lamp'], w=['NEGLAM'])

    def _hg_out(self, oT, sg, h, gain=None, post=1.0, okey='h_oT', sgkey='hsg'):
        S = self.S
        if not hasattr(self, '_hgbufs'):
            self._hgbufs = (self.sb("h_sq", [128, T], BF16), self.sb("h_rstd", [128, T], F32), self.sb("h_ob", [128, T], BF16))
        sq, rstd, ob = self._hgbufs
        S.op('act', lambda e: e.activation(out=sq[:], in_=oT[:], func=AF.Square), r=[okey], w=['h_sq'])
        for (t0, n) in self.tblocks(0, T):
            b = self.bank()
            S.op('pe', lambda e, b=b, t0=t0, n=n: e.matmul(self.ps[b][:, 0:n], lhsT=self.onesb[:], rhs=sq[:, t0:t0 + n], start=True, stop=True),
                 r=['h_sq', 'onesb'], w=[('ps', b)])
            S.op('act', lambda e, b=b, t0=t0, n=n: e.activation(out=rstd[:, t0:t0 + n], in_=self.ps[b][:, 0:n], func=AF.Ln,
                                                               bias=self.epsc[:, 0:1], scale=1.0 / 128.0), r=[('ps', b), 'epsc'], w=['h_rstd'])
        S.op('act', lambda e: e.activation(out=rstd[:], in_=rstd[:], func=AF.Exp, scale=-0.5), r=['h_rstd'], w=['h_rstd'])
        S.op('dve', lambda e: e.tensor_tensor(out=rstd[:], in0=rstd[:], in1=oT[:], op=ALU.mult), r=['h_rstd', okey], w=['h_rstd'])
        if sg is not None:
            S.op('dve', lambda e: e.scalar_tensor_tensor(out=ob[:], in0=rstd[:], scalar=self.pc('hgrn_gain'), in1=sg[:], op0=ALU.mult, op1=ALU.mult),
                 r=['h_rstd', sgkey, 'PC'], w=['h_ob'])
        else:
            S.op('dve', lambda e: e.tensor_scalar(out=ob[:], in0=rstd[:], scalar1=gain, scalar2=post, op0=ALU.mult, op1=ALU.mult),
                 r=['h_rstd', 'PC'], w=['h_ob'])
        S.dma('sp', lambda e, h=h: e.dma_start(out=self.mixT[h * 128:(h + 1) * 128, :], in_=ob[:]), r=['h_ob'], w=[('mixT', h)])
        return ob

    def phase_attn(self):
        S = self.S
        if hasattr(self, '_hgbufs'):
            del self._hgbufs
        MISC = (6, 7)
        ATT_DUMMY = False
        dummy_rhs = self.sb("a_dummy", [128, 512], BF16)
        S.op('dve', lambda e: e.memset(dummy_rhs[:], 0.001), w=['a_dummy'])

        pcg = self.precast_gen(0)
        ropec = self.sb("ropec", [128, TL], F32)
        ropes = self.sb("ropes", [128, TL], F32)
        S.dma('sp', lambda e: e.dma_start(out=ropec[:], in_=self.cst['ropec'][:, :]), w=['ropec'])
        S.dma('sp', lambda e: e.dma_start(out=ropes[:], in_=self.cst['ropes'][:, :]), w=['ropes'])
        tmpc = self.sb("a_tmpc", [128, 128], F32)
        rotb = self.sb("rotb", [128, 128], BF16)
        blkb = self.sb("blkb", [128, 128], BF16)
        for nm, dst in (('rotm', rotb), ('blk1', blkb)):
            S.dma('sp', lambda e, nm=nm: e.dma_start(out=tmpc[:], in_=self.cst[nm][:, :]), w=['a_tmpc'])
            S.op('dve', lambda e, dst=dst: e.tensor_copy(out=dst[:], in_=tmpc[:]), r=['a_tmpc'], w=['a_' + nm])
        X = self.sb("aX", [128, T], F32)
        sq = self.sb("asq", [128, T], BF16)
        rstd = self.sb("arstd", [128, T], F32)
        xnb = self.sb("axnb", [128, TL], BF16)
        t1 = self.sb("at1", [128, 512], F32)
        t2 = self.sb("at2", [128, 512], F32)
        qk = [[self.sb("aqk%d_%d" % (sl, i), [128, T], BF16) for i in range(2)] for sl in range(2)]
        vtm = [self.sb("av%d" % sl, [128, NT, 128], BF16) for sl in range(2)]
        ET = [self.sb("aET%d" % i, [128, 512], BF16) for i in range(4)]
        acc = [self.sb("aacc%d" % i, [128, 512], F32) for i in range(2)]
        tm = [self.sb("atm%d" % i, [128, 512], F32) for i in range(2)]
        rz = [self.sb("arz%d" % i, [128, 512], F32) for i in range(2)]
        aT = [self.sb("aaT%d" % sl, [128, T], F32) for sl in range(2)]
        osq = self.sb("aosq", [128, T], BF16)
        orstd = self.sb("aorstd", [128, T], F32)
        oob = self.sb("aoob", [128, T], BF16)
        post = 1.0 - (0.8 - 0.6 * math.exp(-0.3 * 0))

        def prep_gen(h):
            sl = h % 2
            S.dma('sp', lambda e: e.dma_start(out=vtm[sl][:], in_=self.vtok[8 + h, :, :].rearrange("(n p) c -> p n c", p=128)),
                  r=[('vtok', 8 + (h // 4) * 4, i) for i in range(NT)], w=[('av', sl)])
            for which in range(2):
                row = (40 + which * 8 + h)
                xb = qk[sl][which]
                xk = ('aqk', sl, which)
                gcol = self.pc('gq' if which == 0 else 'gk')
                S.dma('sp', lambda e, row=row: e.dma_start(out=X[:], in_=self.projT[row * 128:(row + 1) * 128, :]), r=[('projT', row)], w=['aX'])
                S.op('act', lambda e: e.activation(out=sq[:], in_=X[:], func=AF.Square), r=['aX'], w=['asq'])
                yield
                for (t0, n) in self.tblocks(0, T):
                    b = self.bank(only=MISC)
                    S.op('pe', lambda e, b=b, t0=t0, n=n: e.matmul(self.ps[b][:, 0:n], lhsT=blkb[:], rhs=sq[:, t0:t0 + n], start=True, stop=True),
                         r=['asq', 'a_blk1'], w=[('ps', b)])
                    S.op('act', lambda e, b=b, t0=t0, n=n: e.activation(out=rstd[:, t0:t0 + n], in_=self.ps[b][:, 0:n], func=AF.Ln,
                                                                       bias=self.epsc[:, 0:1], scale=1.0 / 64.0), r=[('ps', b), 'epsc'], w=['arstd'])
                    yield
                S.op('act', lambda e: e.activation(out=rstd[:], in_=rstd[:], func=AF.Exp, scale=-0.5), r=['arstd'], w=['arstd'])
                yield
                S.op('dve', lambda e, gcol=gcol: e.scalar_tensor_tensor(out=X[:], in0=X[:], scalar=gcol, in1=rstd[:], op0=ALU.mult, op1=ALU.mult),
                     r=['aX', 'arstd', 'PC'], w=['aX'])
                yield
                S.op('pool', lambda e: e.tensor_copy(out=xnb[:], in_=X[:, 0:TL]), r=['aX'], w=['axnb'])
                S.op('pool', lambda e, xb=xb: e.tensor_copy(out=xb[:, TL:T], in_=X[:, TL:T]), r=['aX'], w=[xk])
                yield
                for (t0, n) in self.tblocks(0, TL):
                    b = self.bank(only=MISC)
                    S.op('pe', lambda e, b=b, t0=t0, n=n: e.matmul(self.ps[b][:, 0:n], lhsT=rotb[:], rhs=xnb[:, t0:t0 + n], start=True, stop=True),
                         r=['axnb', 'a_rotm'], w=[('ps', b)])
                    S.op('pool', lambda e, t0=t0, n=n: e.tensor_tensor(out=t2[:, 0:n], in0=X[:, t0:t0 + n], in1=ropec[:, t0:t0 + n], op=ALU.mult),
                         r=['aX', 'ropec'], w=['at2'])
                    S.op('dve', lambda e, b=b, t0=t0, n=n: e.tensor_tensor(out=t1[:, 0:n], in0=self.ps[b][:, 0:n], in1=ropes[:, t0:t0 + n], op=ALU.mult),
                         r=[('ps', b), 'ropes'], w=['at1'])
                    S.op('pool', lambda e, xb=xb, t0=t0, n=n: e.tensor_tensor(out=xb[:, t0:t0 + n], in0=t1[:, 0:n], in1=t2[:, 0:n], op=ALU.add),
                         r=['at1', 'at2'], w=[xk])
                    yield

        def out_gen(h):
            sl = h % 2
            a_ = aT[sl]
            ak = ('aaT', sl)
            S.op('act', lambda e: e.activation(out=osq[:], in_=a_[:], func=AF.Square), r=[ak], w=['aosq'])
            yield
            for (t0, n) in self.tblocks(0, T):
                b = self.bank(only=MISC)
                S.op('pe', lambda e, b=b, t0=t0, n=n: e.matmul(self.ps[b][:, 0:n], lhsT=self.onesb[:], rhs=osq[:, t0:t0 + n], start=True, stop=True),
                     r=['aosq', 'onesb'], w=[('ps', b)])
                S.op('act', lambda e, b=b, t0=t0, n=n: e.activation(out=orstd[:, t0:t0 + n], in_=self.ps[b][:, 0:n], func=AF.Ln,
                                                                   bias=self.epsc[:, 0:1], scale=1.0 / 128.0), r=[('ps', b), 'epsc'], w=['aorstd'])
                yield
            S.op('act', lambda e: e.activation(out=orstd[:], in_=orstd[:], func=AF.Exp, scale=-0.5), r=['aorstd'], w=['aorstd'])
            yield
            S.op('pool', lambda e: e.tensor_tensor(out=orstd[:], in0=orstd[:], in1=a_[:], op=ALU.mult), r=['aorstd', ak], w=['aorstd'])
            yield
            S.op('pool', lambda e: e.tensor_scalar(out=oob[:], in0=orstd[:], scalar1=self.pc('diff_gain'), scalar2=post, op0=ALU.mult, op1=ALU.mult),
                 r=['aorstd', 'PC'], w=['aoob'])
            S.dma('sp', lambda e: e.dma_start(out=self.mixT[(8 + h) * 128:(9 + h) * 128, :], in_=oob[:]), r=['aoob'], w=[('mixT', 8 + h)])
            yield

        etc = [0]

        def main_gen(h):
            sl = h % 2
            qb, kb = qk[sl]
            vt = vtm[sl]
            a_ = aT[sl]
            qblocks = [(t0, n, list(range(NT))) for (t0, n) in self.tblocks(0, TL)] + [(TL, TCX, list(range(TL // 128, NT)))]
            its = []
            for (t0, n, ktiles) in qblocks:
                for ki, kt_ in enumerate(ktiles):
                    its.append(dict(t0=t0, n=n, ki=ki, kt=kt_, nk=len(ktiles)))
            for idx, it in enumerate(its):
                it['idx'] = idx

            def emit_qk(it):
                kt_, t0, n = it['kt'], it['t0'], it['n']
                for m in range(2):
                    bs = 2 + 2 * (it['idx'] % 2) + m
                    S.op('pe', lambda e, bs=bs, m=m, kt_=kt_, t0=t0, n=n: e.matmul(
                        self.ps[bs][:, 0:n], lhsT=kb[m * 64:(m + 1) * 64, kt_ * 128:(kt_ + 1) * 128], rhs=qb[m * 64:(m + 1) * 64, t0:t0 + n],
                        start=True, stop=True), r=[('aqk', sl, 0), ('aqk', sl, 1)], w=[('ps', bs)])

            emit_qk(its[0])
            for idx, it in enumerate(its):
                if idx + 1 < len(its):
                    emit_qk(its[idx + 1])
                t0, n, ki, kt_, nk = it['t0'], it['n'], it['ki'], it['kt'], it['nk']
                ets = []
                for m in range(2):
                    bs = 2 + 2 * (idx % 2) + m
                    et = ET[etc[0] % 4]
                    ek = ('aET', etc[0] % 4)
                    etc[0] += 1
                    ets.append((et, ek))
                    S.op('act', lambda e, bs=bs, et=et, n=n: e.activation(out=et[:, 0:n], in_=self.ps[bs][:, 0:n], func=AF.Exp, scale=0.125),
                         r=[('ps', bs)], w=[ek])
                for m in range(2):
                    et, ek = ets[m]
                    if ATT_DUMMY:
                        S.op('pe', lambda e, kt_=kt_: e.matmul(self.ps[7][:, 0:512], lhsT=vt[:, kt_, :], rhs=dummy_rhs[:, 0:512], start=True, stop=True),
                             r=[('av', sl), 'a_dummy'], w=['ps7_dummy'])
                    S.op('pe', lambda e, m=m, et=et, kt_=kt_, n=n, ki=ki, nk=nk: e.matmul(
                        self.ps[m][:, 0:n], lhsT=vt[:, kt_, :], rhs=et[:, 0:n], start=(ki == 0), stop=(ki == nk - 1)),
                        r=[ek, ('av', sl)], w=[('ps', m)])
                    if ki == 0:
                        S.op('dve', lambda e, m=m, et=et, n=n: e.tensor_copy(out=acc[m][:, 0:n], in_=et[:, 0:n]), r=[ek], w=[('aacc', m)])
                    else:
                        S.op('dve', lambda e, m=m, et=et, n=n: e.tensor_tensor(out=acc[m][:, 0:n], in0=acc[m][:, 0:n], in1=et[:, 0:n], op=ALU.add),
                             r=[ek, ('aacc', m)], w=[('aacc', m)])
                if ki == nk - 1:
                    for m in range(2):
                        bz = self.bank(only=MISC)
                        S.op('pe', lambda e, bz=bz, m=m, n=n: e.matmul(self.ps[bz][:, 0:n], lhsT=self.ones[:], rhs=acc[m][:, 0:n], start=True, stop=True),
                             r=[('aacc', m), 'ones'], w=[('ps', bz)])
                        S.op('act', lambda e, bz=bz, m=m, n=n: e.activation(out=rz[m][:, 0:n], in_=self.ps[bz][:, 0:n], func=AF.Ln), r=[('ps', bz)], w=[('arz', m)])
                        S.op('act', lambda e, m=m, n=n: e.activation(out=rz[m][:, 0:n], in_=rz[m][:, 0:n], func=AF.Exp, scale=-1.0), r=[('arz', m)], w=[('arz', m)])
                        S.op('dve', lambda e, m=m, n=n: e.tensor_tensor(out=tm[m][:, 0:n], in0=self.ps[m][:, 0:n], in1=rz[m][:, 0:n], op=ALU.mult),
                             r=[('ps', m), ('arz', m)], w=[('atm', m)])
                    S.op('dve', lambda e, t0=t0, n=n: e.scalar_tensor_tensor(out=a_[:, t0:t0 + n], in0=tm[1][:, 0:n], scalar=self.NEGLAM[:, 0:1],
                                                                            in1=tm[0][:, 0:n], op0=ALU.mult, op1=ALU.add),
                         r=[('atm', 0), ('atm', 1), 'NEGLAM'], w=[('aaT', sl)])
                yield

        def chain(*gens):
            for g in gens:
                yield from g

        def interleave(main, side, ratio):
            k = 0
            side_done = False
            for _ in main:
                k += 1
                if not side_done and k % ratio == 0:
                    try:
                        next(side)
                    except StopIteration:
                        side_done = True
            if not side_done:
                for _ in side:
                    pass

        for _ in prep_gen(0):
            pass
        def zip_gens(a, b):
            da = db = False
            while not (da and db):
                if not da:
                    try:
                        next(a)
                    except StopIteration:
                        da = True
                if not db:
                    try:
                        next(b)
                    except StopIteration:
                        db = True
                yield

        def take(g, n):
            for _ in range(n):
                try:
                    next(g)
                except StopIteration:
                    return
                yield

        for h in range(8):
            sides = []
            if h >= 1:
                sides.append(out_gen(h - 1))
            if h + 1 < 8:
                sides.append(prep_gen(h + 1))
            interleave(main_gen(h), zip_gens(chain(*sides), take(pcg, 12)), 2)
        for _ in out_gen(7):
            pass
        for _ in pcg:
            pass
        self.free_phase(self.nkeep)

    def rowbcast(self, dst, l, which, ctx):
        S = self.S
        dg = self.sb("rb_diag", [128, 128], F32)
        for q in range(4):
            b = self.bank()
            for j in range(4):
                kc = q * 4 + j
                col = self.mcol(l, which, kc, ctx)
                S.op('dve', lambda e, col=col: e.tensor_scalar(out=dg[:], in0=self.ident[:], scalar1=col, scalar2=None, op0=ALU.mult),
                     r=['ident', ('M', l, which)], w=['rb_diag'])
                S.op('pe', lambda e, b=b, j=j: e.matmul(self.ps[b][:, j * 128:(j + 1) * 128], lhsT=self.ones[:], rhs=dg[:], start=True, stop=True),
                     r=['rb_diag', 'ones'], w=[('ps', b)])
            S.op('act', lambda e, b=b, q=q: e.activation(out=dst[:, q * 512:(q + 1) * 512], in_=self.ps[b][:, :], func=AF.Copy),
                 r=[('ps', b)], w=[('rb', id(dst))])
        return ('rb', id(dst))

    def phase_outproj(self, l, src, w_out, tiles):
        S = self.S
        mT = self.sb("mT", [128, KC, T], BF16)
        wo = self.sb("wo", [128, KC, D], BF16)
        for kc in range(KC):
            if kc % 4 == 0:
                q = kc // 4
                S.dma('pool', lambda e, q=q: e.dma_start(out=wo[:, q * 4:(q + 1) * 4, :],
                                                         in_=w_out[q * 512:(q + 1) * 512, :].rearrange("(k p) f -> p k f", p=128)), w=[('wo', q)])
            S.dma('sp', lambda e, kc=kc: e.dma_start(out=mT[:, kc, :], in_=self.mixT[kc * 128:(kc + 1) * 128, :]),
                  r=[('mixT', kc)], w=[('mT', kc)])
        m2b = [self.sb("m2b%d" % c, [128, D], F32) for c in range(2)]
        m2k = [self.rowbcast(m2b[c], l, 2, c) for c in range(2)]
        xts = [self.sb("op_x%d" % i, [128, D], F32) for i in range(2)]
        tmp = self.sb("op_tmp", [128, 512], F32)
        for n, i in enumerate(tiles):
            ctx = 1 if i >= TL // 128 else 0
            xt = xts[n % 2]
            xk = ('op_x', n % 2)
            S.dma('sp', lambda e, xt=xt, i=i: e.dma_start(out=xt[:], in_=src[i * 128:(i + 1) * 128, :]), r=[('xs', i)], w=[xk])
            bs_ = [self.bank() for _ in range(4)]
            for kc in range(KC):
                for db in range(4):
                    b = bs_[db]
                    S.op('pe', lambda e, b=b, kc=kc, i=i, db=db: e.matmul(self.ps[b][:, :], lhsT=mT[:, kc, i * 128:(i + 1) * 128],
                                                                          rhs=wo[:, kc, db * 512:(db + 1) * 512], start=(kc == 0), stop=(kc == KC - 1)),
                         r=[('mT', kc), ('wo', kc // 4)], w=[('ps', b)])
            for db in range(4):
                b = bs_[db]
                S.op('dve', lambda e, b=b, db=db, ctx=ctx: e.tensor_tensor(out=tmp[:], in0=self.ps[b][:, :], in1=m2b[ctx][:, db * 512:(db + 1) * 512], op=ALU.mult),
                     r=[('ps', b), m2k[ctx]], w=['op_tmp'])
                S.op('pool', lambda e, xt=xt, db=db: e.tensor_tensor(out=xt[:, db * 512:(db + 1) * 512], in0=xt[:, db * 512:(db + 1) * 512], in1=tmp[:], op=ALU.add),
                     r=['op_tmp', xk], w=[xk])
            S.dma('sp', lambda e, xt=xt, i=i: e.dma_start(out=self.xs[i * 128:(i + 1) * 128, :], in_=xt[:]), r=[xk], w=[('xs', i)])
        self.free_phase(self.nkeep)

    def precast_gen(self, l):
        S = self.S
        for e_ in range(16):
            for (src_, dst_) in ((self.moe_w1, self.w1b), (self.moe_w3, self.w3b), (self.moe_w2, self.w2b)):
                S.dma('pool', lambda e, src_=src_, dst_=dst_, e_=e_: e.dma_start(out=dst_[l, e_, :, :], in_=src_[l, e_, :, :]), w=[('wb', id(dst_), l, e_)])
                yield
                yield

    def precast_moe(self, l):
        S = self.S
        for e_ in range(16):
            for (src, dst) in ((self.moe_w1, self.w1b), (self.moe_w3, self.w3b), (self.moe_w2, self.w2b)):
                S.dma('pool', lambda e, src=src, dst=dst, e_=e_: e.dma_start(out=dst[l, e_, :, :], in_=src[l, e_, :, :]), w=[('wb', id(dst), l, e_)])

    def phase_moe(self, l, tiles, src, final=False, extra_side=None):
        S = self.S
        wr = self.sb("moe_wr", [128, KC, 20], F32)
        S.dma('sp', lambda e: e.dma_start(out=wr[:], in_=self.moe_wr[l, :, :].rearrange("(k p) e -> p k e", p=128)), w=['moe_wr'])
        br = self.sb("moe_br", [128, 20], F32)
        S.dma('sp', lambda e: e.dma_start(out=br[:], in_=self.moe_br[l:l + 1, :].partition_broadcast(128)), w=['moe_br'])
        SEL = [self.sb("moe_sel%d" % i, [16, 128], F32) for i in range(2)]
        m5b = [self.sb("m5b%d" % c, [128, D], F32) for c in range(2)]
        m5k = [self.rowbcast(m5b[c], l, 5, c) for c in range(2 if not final else 1)]
        fTs = [self.sb("moe_fT%d" % i, [128, KC, 512], BF16) for i in range(2)]
        h32 = self.sb("moe_h32", [128, KC, 128], F32)
        w1s = [self.sb("moe_w1s%d" % i, [128, KC, 512], BF16) for i in range(2)]
        w3s = [self.sb("moe_w3s%d" % i, [128, KC, 512], BF16) for i in range(2)]
        w2s = [self.sb("moe_w2s", [128, 4, D], BF16)] * 2
        yacc = self.sb("moe_yacc", [128, 4, D], F32)
        hid = self.sb("moe_hid", [128, 4, 512], BF16)
        sbuf_s = [self.sb("moe_s%d" % i, [128, 512], BF16) for i in range(2)]
        sbuf_u = [self.sb("moe_u%d" % i, [128, 512], F32) for i in range(2)]
        cbs = [self.sb("moe_cb%d" % i, [128, 512], F32) for i in range(2)]
        combTs = [self.sb("moe_combT%d" % i, [16, 512], F32) for i in range(2)]
        rt = self.sb("moe_rt", [128, 128], F32)
        xts = self.sb("moe_x", [128, D], F32)
        junk = self.sb("moe_junk", [128, D], BF16)
        wcnt = [0]
        blocks = [tiles[i:i + 4] for i in range(0, len(tiles), 4)]
        def front_gen(bi, btiles):
            fT = fTs[bi % 2]
            combT = combTs[bi % 2]
            fk = ('moe_fT', bi % 2)
            ck = ('combT', bi % 2)
            for ti, i in enumerate(btiles):
                yield from self._nmt_tile(src, i, l, 1, fT, ti, h32, xts, junk, rt, fk)
                b = 7
                for kc in range(KC):
                    S.op('pe', lambda e, b=b, kc=kc: e.matmul(self.ps[b][:, 0:20], lhsT=h32[:, kc, :], rhs=wr[:, kc, :], start=(kc == 0), stop=(kc == KC - 1)),
                         r=['moe_h32', 'moe_wr'], w=[('ps', b)])
                yield
                lg = rt[:, 8:28]
                S.op('dve', lambda e, b=b, lg=lg: e.tensor_tensor(out=lg, in0=self.ps[b][:, 0:20], in1=br[:], op=ALU.add), r=[('ps', b), 'moe_br'], w=['rt_lg'])
                gmax, ngmax, gsum, gw = rt[:, 28:29], rt[:, 29:30], rt[:, 30:31], rt[:, 31:32]
                gmask, pen = rt[:, 32:36], rt[:, 36:40]
                elm, msk1, elm2, msk2 = rt[:, 40:56], rt[:, 56:72], rt[:, 72:88], rt[:, 96:112]
                m1, m2_, dd, w1g, w2g = rt[:, 88:89], rt[:, 89:90], rt[:, 90:91], rt[:, 91:92], rt[:, 92:93]
                gjunk = rt[:, 4:8]
                k_ = 'rt'
                S.op('dve', lambda e: e.tensor_reduce(out=gmax, in_=lg[:, 0:4], axis=AX.X, op=ALU.max), r=['rt_lg'], w=[k_ + '1'])
                S.op('dve', lambda e: e.tensor_scalar(out=ngmax, in0=gmax, scalar1=-1.0, scalar2=None, op0=ALU.mult), r=[k_ + '1'], w=[k_ + '2'])
                S.op('act', lambda e: e.activation(out=gjunk, in_=lg[:, 0:4], func=AF.Exp, bias=ngmax, scale=1.0, accum_out=gsum), r=['rt_lg', k_ + '2'], w=[k_ + '3'])
                S.op('dve', lambda e: e.reciprocal(out=gw, in_=gsum), r=[k_ + '3'], w=[k_ + '4'])
                S.op('dve', lambda e: e.tensor_scalar(out=gmask, in0=lg[:, 0:4], scalar1=gmax, scalar2=None, op0=ALU.is_ge), r=['rt_lg', k_ + '1'], w=[k_ + '5'])
                S.op('dve', lambda e: e.tensor_scalar(out=pen, in0=gmask, scalar1=-1.0, scalar2=1e30, op0=ALU.add, op1=ALU.mult), r=[k_ + '5'], w=[k_ + '6'])
                S.op('dve', lambda e: e.tensor_tensor(out=elm.rearrange("p (g j) -> p g j", j=4), in0=lg[:, 4:20].rearrange("p (g j) -> p g j", j=4),
                                                     in1=pen.unsqueeze(2).to_broadcast([128, 4, 4]), op=ALU.add), r=['rt_lg', k_ + '6'], w=[k_ + '7'])
                S.op('dve', lambda e: e.tensor_reduce(out=m1, in_=elm, axis=AX.X, op=ALU.max), r=[k_ + '7'], w=[k_ + '8'])
                S.op('dve', lambda e: e.tensor_scalar(out=msk1, in0=elm, scalar1=m1, scalar2=None, op0=ALU.is_ge), r=[k_ + '7', k_ + '8'], w=[k_ + '9'])
                S.op('dve', lambda e: e.scalar_tensor_tensor(out=elm2, in0=msk1, scalar=-1e30, in1=elm, op0=ALU.mult, op1=ALU.add), r=[k_ + '9', k_ + '7'], w=[k_ + '10'])
                S.op('dve', lambda e: e.tensor_reduce(out=m2_, in_=elm2, axis=AX.X, op=ALU.max), r=[k_ + '10'], w=[k_ + '11'])
                S.op('dve', lambda e: e.tensor_scalar(out=msk2, in0=elm2, scalar1=m2_, scalar2=None, op0=ALU.is_ge), r=[k_ + '10', k_ + '11', 'rt_lg'], w=[k_ + '12'])
                S.op('dve', lambda e: e.tensor_tensor(out=dd, in0=m2_, in1=m1, op=ALU.subtract), r=[k_ + '11', k_ + '8'], w=[k_ + '13'])
                S.op('act', lambda e: e.activation(out=dd, in_=dd, func=AF.Exp), r=[k_ + '13'], w=[k_ + '13'])
                S.op('dve', lambda e: e.tensor_scalar(out=w1g, in0=dd, scalar1=1.0, scalar2=None, op0=ALU.add), r=[k_ + '13'], w=[k_ + '14'])
                S.op('dve', lambda e: e.reciprocal(out=w1g, in_=w1g), r=[k_ + '14'], w=[k_ + '14'])
                S.op('dve', lambda e: e.tensor_tensor(out=w1g, in0=w1g, in1=gw, op=ALU.mult), r=[k_ + '14', k_ + '4'], w=[k_ + '14'])
                S.op('dve', lambda e: e.tensor_tensor(out=w2g, in0=w1g, in1=dd, op=ALU.mult), r=[k_ + '14', k_ + '13'], w=[k_ + '15'])
                S.op('dve', lambda e: e.tensor_scalar(out=msk1, in0=msk1, scalar1=w1g, scalar2=None, op0=ALU.mult), r=[k_ + '9', k_ + '14'], w=[k_ + '9'])
                S.op('dve', lambda e: e.scalar_tensor_tensor(out=elm, in0=msk2, scalar=w2g, in1=msk1, op0=ALU.mult, op1=ALU.add),
                     r=[k_ + '12', k_ + '15', k_ + '9'], w=[k_ + '7'])
                yield
                b2 = self.bank()
                S.op('pe', lambda e, b2=b2: e.transpose(out=self.ps[b2][0:16, 0:128], in_=elm, identity=self.ident[:]), r=[k_ + '7', 'ident'], w=[('ps', b2)])
                S.op('act', lambda e, b2=b2, ti=ti: e.activation(out=combT[:, ti * 128:(ti + 1) * 128], in_=self.ps[b2][0:16, 0:128], func=AF.Copy),
                     r=[('ps', b2)], w=[ck])
                yield

        def experts_gen(bi, btiles):
            nb = len(btiles) * 128
            ctx = 1 if btiles[0] >= TL // 128 else 0
            fT = fTs[bi % 2]
            combT = combTs[bi % 2]
            fk = ('moe_fT', bi % 2)
            ck = ('combT', bi % 2)
            def emit_comb(e_):
                bc = self.bank()
                sel = SEL[e_ % 2]
                cbb = cbs[e_ % 2]
                S.op('dve', lambda e, sel=sel, e_=e_: e.tensor_copy(out=sel[:], in_=self.ident[0:16, e_:e_ + 1].to_broadcast([16, 128])),
                     r=['ident'], w=[('SEL', e_ % 2)])
                S.op('pe', lambda e, bc=bc, sel=sel, nb=nb: e.matmul(self.ps[bc][:, 0:nb], lhsT=sel[:], rhs=combT[:, 0:nb], start=True, stop=True),
                     r=[('SEL', e_ % 2), ck], w=[('ps', bc)])
                S.op('act', lambda e, bc=bc, nb=nb, cbb=cbb: e.activation(out=cbb[:, 0:nb], in_=self.ps[bc][:, 0:nb], func=AF.Copy), r=[('ps', bc)], w=[('moe_cb', e_ % 2)])

            for e_ in range(16):
                wi = wcnt[0] % 2
                wcnt[0] += 1
                cb = cbs[e_ % 2]
                for half in range(2):
                    S.dma('sp', lambda e, wi=wi, e_=e_, half=half: e.dma_start(out=w1s[wi][:, half * 8:(half + 1) * 8, :],
                          in_=self.w1b[l, e_, half * 1024:(half + 1) * 1024, :].rearrange("(k p) f -> p k f", p=128)),
                          r=[('wb', id(self.w1b), l, e_)], w=[('w1s', wi)])
                    S.dma('sp', lambda e, wi=wi, e_=e_, half=half: e.dma_start(out=w3s[wi][:, half * 8:(half + 1) * 8, :],
                          in_=self.w3b[l, e_, half * 1024:(half + 1) * 1024, :].rearrange("(k p) f -> p k f", p=128)),
                          r=[('wb', id(self.w3b), l, e_)], w=[('w3s', wi)])
                S.dma('sp', lambda e, wi=wi, e_=e_: e.dma_start(out=w2s[wi][:], in_=self.w2b[l, e_, :, :].rearrange("(k p) f -> p k f", p=128)),
                      r=[('wb', id(self.w2b), l, e_)], w=[('w2s', 0)])
                if e_ == 0:
                    emit_comb(0)
                for j in range(4):
                    b1 = self.bank()
                    b3 = self.bank()
                    for (bb, ws, wk) in ((b1, w1s[wi], ('w1s', wi)), (b3, w3s[wi], ('w3s', wi))):
                        for kc in range(KC):
                            S.op('pe', lambda e, bb=bb, ws=ws, kc=kc, j=j, nb=nb: e.matmul(
                                self.ps[bb][:, 0:nb], lhsT=ws[:, kc, j * 128:(j + 1) * 128], rhs=fT[:, kc, 0:nb], start=(kc == 0), stop=(kc == KC - 1)),
                                r=[wk, fk], w=[('ps', bb)])
                    ss, uu = sbuf_s[j % 2], sbuf_u[j % 2]
                    S.op('act', lambda e, b1=b1, ss=ss, nb=nb: e.activation(out=ss[:, 0:nb], in_=self.ps[b1][:, 0:nb], func=AF.Silu),
                         r=[('ps', b1)], w=[('moe_s', j % 2)])
                    S.op('dve', lambda e, b3=b3, uu=uu, nb=nb, cb=cb: e.tensor_tensor(out=uu[:, 0:nb], in0=self.ps[b3][:, 0:nb], in1=cb[:, 0:nb], op=ALU.mult),
                         r=[('ps', b3), ('moe_cb', e_ % 2)], w=[('moe_u', j % 2)])
                    S.op('pool', lambda e, ss=ss, uu=uu, j=j, nb=nb: e.tensor_tensor(out=hid[:, j, 0:nb], in0=ss[:, 0:nb], in1=uu[:, 0:nb], op=ALU.mult),
                         r=[('moe_s', j % 2), ('moe_u', j % 2)], w=[('moe_hid', j)])
                    if j == 2 and e_ + 1 < 16:
                        emit_comb(e_ + 1)
                    yield
                for ti in range(len(btiles)):
                    for db in range(4):
                        b = self.bank()
                        for j in range(4):
                            S.op('pe', lambda e, b=b, j=j, ti=ti, db=db, wi=wi: e.matmul(
                                self.ps[b][:, :], lhsT=hid[:, j, ti * 128:(ti + 1) * 128], rhs=w2s[wi][:, j, db * 512:(db + 1) * 512],
                                start=(j == 0), stop=(j == 3)), r=[('moe_hid', j), ('w2s', 0)], w=[('ps', b)])
                        ya = yacc[:, ti, db * 512:(db + 1) * 512]
                        yk = ('yacc', ti, db)
                        if e_ == 0:
                            S.op('act', lambda e, b=b, ya=ya: e.activation(out=ya, in_=self.ps[b][:, :], func=AF.Copy), r=[('ps', b)], w=[yk])
                        else:
                            S.op('dve', lambda e, b=b, ya=ya: e.tensor_tensor(out=ya, in0=self.ps[b][:, :], in1=ya, op=ALU.add), r=[('ps', b), yk], w=[yk])
            for ti, i in enumerate(btiles):
                S.dma('sp', lambda e, i=i: e.dma_start(out=xts[:], in_=src[i * 128:(i + 1) * 128, :]), r=[('xs', i)], w=['moe_x'])
                for db in range(4):
                    S.op('dve', lambda e, ti=ti, db=db, ctx=ctx: e.tensor_tensor(out=yacc[:, ti, db * 512:(db + 1) * 512], in0=yacc[:, ti, db * 512:(db + 1) * 512],
                                                                            in1=m5b[ctx][:, db * 512:(db + 1) * 512], op=ALU.mult),
                         r=[('yacc', ti, db), m5k[ctx]], w=[('yacc', ti, db)])
                    S.op('pool', lambda e, ti=ti, db=db: e.tensor_tensor(out=yacc[:, ti, db * 512:(db + 1) * 512], in0=yacc[:, ti, db * 512:(db + 1) * 512],
                                                                      in1=xts[:, db * 512:(db + 1) * 512], op=ALU.add),
                         r=[('yacc', ti, db), 'moe_x'], w=[('yacc', ti, db)])
                dst = self.out if final else self.xs
                S.dma('sp', lambda e, dst=dst, ti=ti, i=i: e.dma_start(out=dst[i * 128:(i + 1) * 128, :], in_=yacc[:, ti, :]),
                      r=[('yacc', ti, db) for db in range(4)], w=[('xs', i) if not final else ('out', i)])
            yield

        def interleave(main, side):
            side_done = side is None
            for _ in main:
                if not side_done:
                    try:
                        next(side)
                    except StopIteration:
                        side_done = True
            if not side_done:
                for _ in side:
                    pass

        self.bank_reserved = (7,)
        for _ in front_gen(0, blocks[0]):
            pass
        def chain2(a, b, n):
            if a is not None:
                yield from a
            if b is not None:
                for _ in range(n):
                    try:
                        next(b)
                    except StopIteration:
                        return
                    yield

        for bi, btiles in enumerate(blocks):
            side = front_gen(bi + 1, blocks[bi + 1]) if bi + 1 < len(blocks) else None
            interleave(experts_gen(bi, btiles), chain2(side, extra_side, 40))
        if extra_side is not None:
            for _ in extra_side:
                pass
        self.bank_reserved = ()
        self.free_phase(self.nkeep)

    def _nmt_tile(self, src, i, l, nrm, hT, ti, h32, xt, junk, st, fk):
        S = self.S
        shidx = 0 if nrm == 0 else 3
        ctx = 1 if i >= TL // 128 else 0
        S.dma('sp', lambda e: e.dma_start(out=xt[:], in_=src[i * 128:(i + 1) * 128, :]), r=[('xs', i)], w=['moe_x'])
        ss, rs = st[:, 0:1], st[:, 1:2]
        S.op('act', lambda e: e.activation(out=junk[:], in_=xt[:], func=AF.Square, accum_out=ss), r=['moe_x'], w=['nmt_ss', 'moe_junk'])
        S.op('act', lambda e: e.activation(out=ss, in_=ss, func=AF.Sqrt, bias=self.epsc[:, 0:1], scale=1.0 / D), r=['nmt_ss', 'epsc'], w=['nmt_ss'])
        S.op('dve', lambda e: e.reciprocal(out=rs, in_=ss), r=['nmt_ss'], w=['nmt_rs'])
        S.op('dve', lambda e: e.tensor_scalar(out=xt[:], in0=xt[:], scalar1=rs, scalar2=None, op0=ALU.mult), r=['nmt_rs', 'moe_x'], w=['moe_x'])
        yield
        gs = self.GS[(l, nrm, ctx)]
        for q in range(4):
            b = self.bank()
            for j in range(4):
                kc = q * 4 + j
                S.op('pe', lambda e, b=b, j=j, kc=kc: e.transpose(out=self.ps[b][:, j * 128:(j + 1) * 128], in_=xt[:, kc * 128:(kc + 1) * 128],
                                                                  identity=self.ident[:]), r=['moe_x', 'ident'], w=[('ps', b)])
            for j in range(4):
                kc = q * 4 + j
                sh = self.mcol(l, shidx, kc, ctx)
                if j % 2 == 0:
                    S.op('act', lambda e, b=b, j=j, kc=kc, sh=sh: e.activation(out=h32[:, kc, :], in_=self.ps[b][:, j * 128:(j + 1) * 128], func=AF.Identity,
                                                                              bias=sh, scale=gs[:, kc:kc + 1]), r=[('ps', b), ('GS', l, nrm, ctx), ('M', l, shidx)], w=['moe_h32'])
                else:
                    S.op('dve', lambda e, b=b, j=j, kc=kc, sh=sh: e.tensor_scalar(out=h32[:, kc, :], in0=self.ps[b][:, j * 128:(j + 1) * 128],
                                                                                 scalar1=gs[:, kc:kc + 1], scalar2=sh, op0=ALU.mult, op1=ALU.add),
                         r=[('ps', b), ('GS', l, nrm, ctx), ('M', l, shidx)], w=['moe_h32'])
        yield
        S.op('pool', lambda e: e.tensor_copy(out=hT[:, :, ti * 128:(ti + 1) * 128], in_=h32[:, :, :]), r=['moe_h32'], w=[fk])

    def phase_inproj1(self):
        S = self.S
        hT = self.sb("hT1", [128, KC, T], BF16)
        self.nmt(self.xs, list(range(NT)), 1, 0, hT, tag="b")
        stg = [self.sb("stg1_%d" % i, [128, T], F32) for i in range(2)]
        tmp = self.sb("stg1_tmp", [128, T], F32)
        cnt = [0]

        def evac(g, t0, n, pap, b):
            st = stg[cnt[0] % 2]
            S.op('act', lambda e, st=st, t0=t0, n=n, pap=pap: e.activation(out=st[:, t0:t0 + n], in_=pap, func=AF.Copy),
                 r=[('ps', b)], w=[('stg', cnt[0] % 2)])

        def done(g):
            st = stg[cnt[0] % 2]
            sk = ('stg', cnt[0] % 2)
            if g < 16:
                S.op('act', lambda e, st=st: e.activation(out=tmp[:], in_=st[:], func=AF.Square), r=[sk], w=['stg_tmp'])
                S.op('dve', lambda e: e.tensor_scalar(out=tmp[:], in0=tmp[:], scalar1=0.044715, scalar2=1.0, op0=ALU.mult, op1=ALU.add),
                     r=['stg_tmp'], w=['stg_tmp'])
                S.op('pool', lambda e, st=st: e.tensor_tensor(out=tmp[:], in0=tmp[:], in1=st[:], op=ALU.mult), r=['stg_tmp', sk], w=['stg_tmp'])
                S.op('act', lambda e: e.activation(out=tmp[:], in_=tmp[:], func=AF.Sigmoid, scale=2.0 * math.sqrt(2.0 / math.pi)),
                     r=['stg_tmp'], w=['stg_tmp'])
                S.op('dve', lambda e, st=st: e.tensor_tensor(out=st[:], in0=st[:], in1=tmp[:], op=ALU.mult), r=['stg_tmp', sk], w=[sk])
            S.dma('sp', lambda e, st=st, g=g: e.dma_start(out=self.projT[g * 128:(g + 1) * 128, :], in_=st[:]), r=[sk], w=[('projT', g)])
            cnt[0] += 1

        groups = [[q * 4 + j for j in range(4)] for q in range(8)]
        self.linear_fm(self.w_in1, groups, hT, "b", evac, done)
        self.free_phase(self.nkeep)

    def phase_rglru(self):
        S = self.S
        GW = self.sb("rg_GW", [128, 64, 128], BF16)
        for q in range(4):
            S.dma('pool', lambda e, q=q: e.dma_start(out=GW[:, q * 16:(q + 1) * 16, :],
                                                     in_=self.gate_w[q // 2, q % 2, :, :, :].rearrange("h i j -> i h j")), w=[('GW', q)])
        CL = self.sb("rg_CL", [128, 32], F32)
        S.op('act', lambda e: e.activation(out=CL[:], in_=self.pc('rg_lam'), func=AF.Exp, scale=-1.0), r=['PC'], w=['CL'])
        S.op('act', lambda e: e.activation(out=CL[:], in_=CL[:], func=AF.Ln, bias=self.ones[:, 0:1], scale=1.0), r=['CL', 'ones'], w=['CL'])
        S.op('dve', lambda e: e.tensor_scalar(out=CL[:], in0=CL[:], scalar1=-8.0, scalar2=None, op0=ALU.mult), r=['CL'], w=['CL'])
        ygs = [self.sb("rg_y%d" % i, [128, T], F32) for i in range(2)]
        us = [self.sb("rg_u%d" % i, [128, T], F32) for i in range(2)]
        uc = self.sb("rg_uc", [128, T], F32)
        ucb = self.sb("rg_ucb", [128, T], BF16)
        rr = self.sb("rg_r", [128, T], F32)
        ig = self.sb("rg_i", [128, T], F32)
        a = self.sb("rg_a", [128, T], F32)
        tmp = self.sb("rg_tmp", [128, T], F32)
        bx = self.sb("rg_bx", [128, T], F32)
        hh = [self.sb("rg_h%d" % i, [128, T], F32) for i in range(2)]
        ob = self.sb("rg_ob", [128, TL], BF16)
        segs = [(0, TL), (TL, T)]
        o_cw, _ = PV_OFF['conv_w']
        o_cb, _ = PV_OFF['conv_b']
        o_gb, _ = PV_OFF['gate_b']
        def load(j):
            yg_, u_ = ygs[j % 2], us[j % 2]
            S.dma('sp', lambda e: e.dma_start(out=yg_[:], in_=self.projT[j * 128:(j + 1) * 128, :]), r=[('projT', j)], w=[('rg_y', j % 2)])
            S.dma('sp', lambda e: e.dma_start(out=u_[:], in_=self.projT[(16 + j) * 128:(17 + j) * 128, :]), r=[('projT', 16 + j)], w=[('rg_u', j % 2)])

        def chunk(j, yg, u):
            yk, uk = ('rg_y', j % 2), ('rg_u', j % 2)
            if j + 1 < 16:
                load(j + 1)
            wcol = lambda i, j=j: self.PC[:, o_cw + i * 16 + j:o_cw + i * 16 + j + 1]
            bcol = self.PC[:, o_cb + j:o_cb + j + 1]
            S.op('act', lambda e, wc=wcol(2), bcol=bcol: e.activation(out=uc[:], in_=u[:], func=AF.Identity, bias=bcol, scale=wc), r=[uk, 'PC'], w=['rg_uc'])
            for (s0, s1) in segs:
                for (tap, sh) in ((0, -2), (1, -1), (3, 1)):
                    if sh < 0:
                        o0, o1, i0, i1 = s0 - sh, s1, s0, s1 + sh
                    else:
                        o0, o1, i0, i1 = s0, s1 - sh, s0 + sh, s1
                    S.op('dve', lambda e, wc=wcol(tap), o0=o0, o1=o1, i0=i0, i1=i1: e.scalar_tensor_tensor(
                        out=uc[:, o0:o1], in0=u[:, i0:i1], scalar=wc, in1=uc[:, o0:o1], op0=ALU.mult, op1=ALU.add), r=[uk, 'rg_uc', 'PC'], w=['rg_uc'])
            S.op('act', lambda e: e.activation(out=ucb[:], in_=uc[:], func=AF.Copy), r=['rg_uc'], w=['rg_ucb'])
            for d_ in range(2):
                for g_ in range(2):
                    dst = rr if g_ == 0 else ig
                    dk = 'rg_r' if g_ == 0 else 'rg_i'
                    idx = (d_ * 2 + g_) * 16 + j
                    gb = self.PC[:, o_gb + idx:o_gb + idx + 1]
                    for (t0, n) in self.tblocks(0, T):
                        b = self.bank()
                        S.op('pe', lambda e, b=b, idx=idx, t0=t0, n=n: e.matmul(self.ps[b][:, 0:n], lhsT=GW[:, idx, :], rhs=ucb[:, t0:t0 + n], start=True, stop=True),
                             r=[('GW', idx // 16), 'rg_ucb'], w=[('ps', b)])
                        S.op('act', lambda e, b=b, dst=dst, gb=gb, t0=t0, n=n: e.activation(out=dst[:, t0:t0 + n], in_=self.ps[b][:, 0:n], func=AF.Sigmoid, bias=gb, scale=1.0),
                             r=[('ps', b), 'PC'], w=[dk])
                clc = CL[:, d_ * 16 + j:d_ * 16 + j + 1]
                S.op('act', lambda e, clc=clc: e.activation(out=a[:], in_=rr[:], func=AF.Exp, scale=clc), r=['rg_r', 'CL'], w=['rg_a'])
                S.op('act', lambda e: e.activation(out=tmp[:], in_=a[:], func=AF.Square), r=['rg_a'], w=['rg_tmp'])
                S.op('act', lambda e: e.activation(out=tmp[:], in_=tmp[:], func=AF.Sqrt, bias=self.ones[:, 0:1], scale=-1.0), r=['rg_tmp', 'ones'], w=['rg_tmp'])
                S.op('dve', lambda e: e.tensor_tensor(out=bx[:], in0=tmp[:], in1=ig[:], op=ALU.mult), r=['rg_tmp', 'rg_i'], w=['rg_bx'])
                S.op('pool', lambda e: e.tensor_tensor(out=bx[:], in0=bx[:], in1=uc[:], op=ALU.mult), r=['rg_bx', 'rg_uc'], w=['rg_bx'])
                h_ = hh[d_]
                hk = ('rg_h', d_)
                if d_ == 0:
                    S.op('dve', lambda e, h_=h_: e.tensor_tensor_scan(out=h_[:, TL:T], data0=a[:, TL:T], data1=bx[:, TL:T], initial=0.0, op0=ALU.mult, op1=ALU.add),
                         r=['rg_a', 'rg_bx'], w=[hk])
                    S.op('dve', lambda e, h_=h_: e.tensor_tensor_scan(out=h_[:, 0:TL], data0=a[:, 0:TL], data1=bx[:, 0:TL], initial=h_[:, T - 1:T], op0=ALU.mult, op1=ALU.add),
                         r=['rg_a', 'rg_bx', hk], w=[hk])
                else:
                    S.op('dve', lambda e, h_=h_: e.tensor_tensor_scan(out=h_[:, TL:T][:, ::-1], data0=a[:, TL:T][:, ::-1], data1=bx[:, TL:T][:, ::-1], initial=0.0,
                                                                     op0=ALU.mult, op1=ALU.add), r=['rg_a', 'rg_bx'], w=[hk])
                    S.op('dve', lambda e, h_=h_: e.tensor_tensor_scan(out=h_[:, 0:TL][:, ::-1], data0=a[:, 0:TL][:, ::-1], data1=bx[:, 0:TL][:, ::-1],
                                                                     initial=h_[:, TL:TL + 1], op0=ALU.mult, op1=ALU.add), r=['rg_a', 'rg_bx', hk], w=[hk])
            S.op('pool', lambda e: e.tensor_tensor(out=tmp[:, 0:TL], in0=hh[0][:, 0:TL], in1=hh[1][:, 0:TL], op=ALU.add), r=[('rg_h', 0), ('rg_h', 1)], w=['rg_tmp'])
            S.op('dve', lambda e: e.tensor_tensor(out=ob[:], in0=tmp[:, 0:TL], in1=yg[:, 0:TL], op=ALU.mult), r=['rg_tmp', yk], w=['rg_ob'])
            S.dma('sp', lambda e, j=j: e.dma_start(out=self.mixT[j * 128:(j + 1) * 128, 0:TL], in_=ob[:]), r=['rg_ob'], w=[('mixT', j)])
        load(0)
        for j in range(16):
            chunk(j, ygs[j % 2], us[j % 2])
        self.free_phase(self.nkeep)

    def build(self):
        self.phase_prep()
        if self.upto >= 1:
            self.phase_inproj0()
        if self.upto >= 2:
            self.phase_hgrn()
        if self.upto >= 3:
            self.phase_attn()
        if self.upto >= 4:
            self.phase_outproj(0, self.xin, self.w_out0, list(range(NT)))
        if self.upto >= 5:
            self.phase_moe(0, list(range(NT)), self.xs, extra_side=self.precast_gen(1))
        if self.upto >= 6:
            self.phase_inproj1()
        if self.upto >= 7:
            self.phase_rglru()
        if self.upto >= 8:
            self.phase_outproj(1, self.xs, self.w_out1, list(range(TL // 128)))
        if self.upto >= 9:
            self.phase_moe(1, list(range(TL // 128)), self.xs, final=True)
        if self.debug:
            S = self.S
            S.op('dve', lambda e: e.tensor_copy(out=self.dbgt[:, 0:192], in_=self.M[0][:].rearrange("p f t -> p (f t)")),
                 r=[('M', 0)], w=['dbgt']) if False else None
        n = self.S.finalize()
        return n


def make_in_maps(inp, cores):
    consts = host_consts()
    shared = {
        'ada_w': np.ascontiguousarray(inp['ada_w'], np.float32),
        'w_in0': np.ascontiguousarray(inp['even_w_in'][0], np.float32),
        'w_out0': np.ascontiguousarray(inp['even_w_out'][0], np.float32),
        'w_in1': np.ascontiguousarray(inp['odd_w_in'][0], np.float32),
        'w_out1': np.ascontiguousarray(inp['odd_w_out'][0], np.float32),
        'gate_w': np.ascontiguousarray(inp['rg_gate_w'][0], np.float32),
        'moe_wr': np.ascontiguousarray(np.concatenate([inp['moe_w_grp'], inp['moe_w_exp']], axis=-1), np.float32),
        'moe_br': np.ascontiguousarray(np.concatenate([inp['moe_b_grp'], inp['moe_b_exp']], axis=-1), np.float32),
        'moe_w1': np.ascontiguousarray(inp['moe_w1'], np.float32),
        'moe_w3': np.ascontiguousarray(inp['moe_w3'], np.float32),
        'moe_w2': np.ascontiguousarray(inp['moe_w2'], np.float32),
    }
    for k, v in consts.items():
        shared['c_' + k] = v
    maps = []
    for b in cores:
        m = dict(shared)
        m['xin'] = np.ascontiguousarray(np.concatenate([inp['x'][b], inp['ctx'][b]], axis=0), np.float32)
        m['pvec'] = pack_pvec(inp, b)
        maps.append(m)
    return maps


def run(inp, debug=False, upto=99, cores=None):
    if cores is None:
        cores = list(range(NCORES))
    p = Prog(debug=debug, upto=upto)
    import time as _t
    _t0 = _t.time()
    n = p.build()
    print("instructions:", n, "build_s", round(_t.time() - _t0, 1), flush=True)
    maps = make_in_maps(inp, cores)
    _t0 = _t.time()
    res = run_bass_kernel_spmd(p.nc, maps, core_ids=list(range(len(cores))))
    print("compile+run_s", round(_t.time() - _t0, 1), flush=True)
    return res.results


def kernel(**inputs):
    inp = {k: np.asarray(v) for k, v in inputs.items()}
    res = run(inp)
    out = np.stack([np.asarray(r['out'], np.float32) for r in res], axis=0)
    return out
```

```python
import math
import numpy as np
import ml_dtypes
import concourse.bass as bass
import concourse.mybir as mybir
from concourse.bass_utils import run_bass_kernel_spmd

F32 = mybir.dt.float32
BF16 = mybir.dt.bfloat16
U8 = mybir.dt.uint8
ALU = mybir.AluOpType
AF = mybir.ActivationFunctionType
AX = mybir.AxisListType

D = 2048
KC = 16
TL = 2048
TCX = 256
T = TL + TCX
NT = T // 128
EPS = 1e-6
NCORES = 8
CH = 64
NCHK = T // CH


class Sched:
    CE = ('pe', 'act', 'dve', 'pool')

    def __init__(self, nc):
        self.nc = nc
        self.eng = {'pe': nc.tensor, 'act': nc.scalar, 'dve': nc.vector, 'pool': nc.gpsimd, 'sp': nc.sync}
        self.ops = []
        self.wr = {}
        self.rd = {}
        self.esem = {e: nc.alloc_semaphore(name='e_' + e) for e in self.CE}
        npool = {'sp': 40, 'pool': 40, 'act': 4}
        self.dsem = {q: [nc.alloc_semaphore(name='d_%s%d' % (q, i)) for i in range(n)] for q, n in npool.items()}

    def _deps(self, i, eng, isdma, r, w):
        deps = []
        for k in r:
            st = self.wr.get(k)
            if st:
                for p in st.values():
                    deps.append((p, 'RAW'))
        for k in w:
            st = self.wr.get(k)
            rs = self.rd.get(k)
            if rs:
                for p in rs.values():
                    deps.append((p, 'WAR'))
            if st:
                for p in st.values():
                    deps.append((p, 'WAW'))
        tag = ('dma', i) if isdma else eng
        for k in r:
            self.rd.setdefault(k, {})[tag] = i
        for k in w:
            if self.rd.get(k):
                self.rd[k] = {}
                self.wr[k] = {tag: i}
            else:
                self.wr.setdefault(k, {})[tag] = i
        return deps

    def op(self, eng, fn, r=(), w=()):
        i = len(self.ops)
        self.ops.append(dict(eng=eng, fn=fn, dma=False, deps=self._deps(i, eng, False, r, w), inc=False))
        return i

    def dma(self, q, fn, r=(), w=()):
        i = len(self.ops)
        self.ops.append(dict(eng=q, fn=fn, dma=True, deps=self._deps(i, q, True, r, w), inc=True))
        return i

    def barrier(self):
        self.ops.append(dict(barrier=True))
        self.wr = {}
        self.rd = {}

    def finalize(self):
        ops = self.ops
        last = {e: None for e in self.CE}
        for i, o in enumerate(ops):
            if o.get('barrier'):
                for e in self.CE:
                    if last[e] is not None:
                        ops[last[e]]['inc'] = True
                continue
            need = []
            for (p, kind) in o['deps']:
                po = ops[p]
                if po['dma']:
                    need.append(p)
                elif po['eng'] == o['eng'] and not o['dma']:
                    if o['eng'] == 'pe' or (kind != 'RAW' and o['eng'] != 'pool'):
                        continue
                    need.append(p)
                else:
                    need.append(p)
            o['need'] = need
            for p in need:
                ops[p]['inc'] = True
            if not o['dma']:
                last[o['eng']] = i
        for e in self.CE:
            if last[e] is not None:
                ops[last[e]]['inc'] = True
        cnt = {e: 0 for e in self.CE}
        duse = {q: 0 for q in self.dsem}
        dval = {}
        dlast = {}
        for i, o in enumerate(ops):
            if o.get('barrier'):
                continue
            if o['dma']:
                q = o['eng']
                pool = self.dsem[q]
                s = pool[duse[q] % len(pool)]
                duse[q] += 1
                key = id(s)
                dval[key] = dval.get(key, 0) + 16
                o['sem'] = s
                o['val'] = dval[key]
                o['prev'] = dlast.get(key)
                dlast[key] = i
            elif o['inc']:
                cnt[o['eng']] += 1
                o['sem'] = self.esem[o['eng']]
                o['val'] = cnt[o['eng']]
        waited = {e: {} for e in self.eng}
        cur = {e: 0 for e in self.CE}
        dcur = {}

        def wait(e, s, v):
            wd = waited[e]
            if wd.get(id(s), 0) >= v:
                return
            wd[id(s)] = v
            self.eng[e].wait_ge(s, v)

        nins = 0
        for i, o in enumerate(ops):
            if o.get('barrier'):
                for e in self.eng:
                    for e2 in self.CE:
                        if cur[e2] > 0:
                            wait(e, self.esem[e2], cur[e2])
                    for key, (s, v) in dcur.items():
                        wait(e, s, v)
                continue
            e = o['eng']
            for p in o['need']:
                po = ops[p]
                wait(e, po['sem'], po['val'])
            if o['dma'] and o['prev'] is not None:
                po = ops[o['prev']]
                wait(e, po['sem'], po['val'])
            ins = o['fn'](self.eng[e])
            nins += 1
            if o['dma']:
                ins.then_inc(o['sem'], 16)
                dcur[id(o['sem'])] = (o['sem'], o['val'])
            elif o['inc']:
                ins.then_inc(o['sem'], 1)
                cur[e] = o['val']
        for e in self.eng:
            for e2 in self.CE:
                if cur[e2] > 0:
                    wait(e, self.esem[e2], cur[e2])
            for key, (s, v) in dcur.items():
                wait(e, s, v)
        return nins


PV_LAYOUT = [
    ('c', 16), ('c_ctx', 16), ('ada_b0', 96), ('ada_b1', 96),
    ('norm_mix0', 16), ('norm_mix1', 16), ('norm_ffn0', 16), ('norm_ffn1', 16),
    ('lbl_f0', 8), ('lbl_f1', 8), ('lbl_b0', 8), ('lbl_b1', 8),
    ('hgrn_gain', 1), ('gq', 1), ('gk', 1), ('dl_a', 1), ('dl_b', 1), ('diff_gain', 1),
    ('conv_w', 64), ('conv_b', 16), ('gate_b', 64), ('rg_lam', 32),
]
PV_OFF = {}
_o = 0
for _n, _k in PV_LAYOUT:
    PV_OFF[_n] = (_o, _k)
    _o += _k
PV_ROWS = ((_o + 127) // 128) * 128
NPG = PV_ROWS // 128


def pack_pvec(inp, b):
    rows = np.zeros((PV_ROWS, 128), np.float32)

    def put(name, arr):
        o, k = PV_OFF[name]
        rows[o:o + k] = np.asarray(arr, np.float32).reshape(k, 128)

    put('c', inp['c'][b])
    put('c_ctx', inp['c_ctx'])
    put('ada_b0', inp['ada_b'][0])
    put('ada_b1', inp['ada_b'][1])
    for l in range(2):
        put('norm_mix%d' % l, inp['norm_mix'][l])
        put('norm_ffn%d' % l, inp['norm_ffn'][l])
    lbl = inp['hgrn_lb_logits']
    put('lbl_f0', lbl[0, 0]); put('lbl_f1', lbl[0, 1]); put('lbl_b0', lbl[1, 0]); put('lbl_b1', lbl[1, 1])
    put('hgrn_gain', inp['hgrn_out_norm'][0])
    put('gq', np.concatenate([inp['diff_qk_norm'][0, 0]] * 2))
    put('gk', np.concatenate([inp['diff_qk_norm'][0, 1]] * 2))
    dl = inp['diff_lambda'][0]
    put('dl_a', np.concatenate([dl[0], dl[2]]))
    put('dl_b', np.concatenate([dl[1], dl[3]]))
    put('diff_gain', inp['diff_out_norm'][0])
    put('conv_w', inp['odd_conv_w'][0])
    put('conv_b', inp['odd_conv_b'][0])
    put('gate_b', inp['rg_gate_b'][0])
    put('rg_lam', inp['rg_lambda'][0])
    return rows


def host_consts():
    c = {}
    c['ident'] = np.eye(128, dtype=np.float32)
    s = np.arange(128)[:, None]
    t = np.arange(128)[None, :]
    same = (s // CH) == (t // CH)
    c['maskf'] = (same & (s <= t)).astype(np.float32)
    c['maskb'] = (same & (s >= t)).astype(np.float32)
    m01 = np.ones((128, T + 1), np.float32)
    m01[:, 0::CH] = 0.0
    c['m01'] = m01
    n = TL
    row = (np.arange(n) // 64).astype(np.float32)
    col = (np.arange(n) % 64).astype(np.float32)
    pairs = 16
    inv = (10000.0 ** (-np.arange(pairs, dtype=np.float32) / pairs)).astype(np.float32)
    ang = np.concatenate([row[:, None] * inv, col[:, None] * inv], axis=-1).astype(np.float32)
    cosv = np.cos(ang).astype(np.float32)
    sinv = np.sin(ang).astype(np.float32)
    pidx = (np.arange(128) % 64) // 2
    c['ropec'] = np.ascontiguousarray(cosv[:, pidx].T)
    c['ropes'] = np.ascontiguousarray(sinv[:, pidx].T)
    rm = np.zeros((128, 128), np.float32)
    for i in range(64):
        rm[2 * i + 1, 2 * i] = -1.0
        rm[2 * i, 2 * i + 1] = 1.0
    c['rotm'] = rm
    bo = np.zeros((128, 128), np.float32)
    bo[:64, :64] = 1.0
    bo[64:, 64:] = 1.0
    c['blk1'] = bo
    hm = np.zeros((128, 2), np.float32)
    hm[:64, 0] = 1.0
    hm[64:, 1] = 1.0
    c['halfm'] = hm
    return c


CONST_SHAPES = {'ident': [128, 128], 'maskf': [128, 128], 'maskb': [128, 128], 'm01': [128, T + 1],
                'ropec': [128, TL], 'ropes': [128, TL], 'rotm': [128, 128], 'blk1': [128, 128], 'halfm': [128, 2]}


class Prog:
    def __init__(self, debug=False, upto=99):
        self.debug = debug
        self.upto = upto
        nc = bass.Bass("TRN2", target_bir_lowering=False)
        self.nc = nc
        self.S = Sched(nc)
        self.live = []
        self._uid = 0
        kin = "ExternalInput"
        dt = lambda name, shape, dtype, kind: nc.dram_tensor(name, shape, dtype, kind=kind).ap()
        self.xin = dt("xin", [T, D], F32, kin)
        self.pvec = dt("pvec", [PV_ROWS, 128], F32, kin)
        self.cst = {k: dt("c_" + k, shp, F32, kin) for k, shp in CONST_SHAPES.items()}
        self.ada_w = dt("ada_w", [2, D, 6 * D], F32, kin)
        self.w_in0 = dt("w_in0", [D, 8192], F32, kin)
        self.w_out0 = dt("w_out0", [D, D], F32, kin)
        self.w_in1 = dt("w_in1", [D, 4096], F32, kin)
        self.w_out1 = dt("w_out1", [D, D], F32, kin)
        self.gate_w = dt("gate_w", [2, 2, 16, 128, 128], F32, kin)
        self.moe_wr = dt("moe_wr", [2, D, 20], F32, kin)
        self.moe_br = dt("moe_br", [2, 20], F32, kin)
        self.moe_w1 = dt("moe_w1", [2, 16, D, 512], F32, kin)
        self.moe_w3 = dt("moe_w3", [2, 16, D, 512], F32, kin)
        self.moe_w2 = dt("moe_w2", [2, 16, 512, D], F32, kin)
        self.out = dt("out", [TL, D], F32, "ExternalOutput")
        sk = "ExternalOutput" if debug else "Internal"
        self.xs = dt("xs", [T, D], F32, sk)
        self.projT = dt("projT", [8192, T], F32, sk)
        self.vtok = dt("vtok", [16, T, 128], BF16, sk)
        self.mixT = dt("mixT", [D, T], BF16, sk)
        self.w1b = dt("w1b", [2, 16, D, 512], BF16, "Internal")
        self.w3b = dt("w3b", [2, 16, D, 512], BF16, "Internal")
        self.w2b = dt("w2b", [2, 16, 512, D], BF16, "Internal")
        if debug:
            self.dbg = dt("dbg", [128, 2048], F32, "ExternalOutput")
        self.ps = [nc.alloc_psum_tensor("ps%d" % i, [128, 512], F32) for i in range(8)]
        self.psi = 0

    def sb(self, name, shape, dtype):
        g = self.nc.sbuf_tensor(name + "_%d" % self._uid, shape, dtype)
        t = g.__enter__()
        self._uid += 1
        self.live.append(g)
        return t

    def free_phase(self, keep=0):
        self.S.barrier()
        while len(self.live) > keep:
            self.live.pop().__exit__(None, None, None)

    def bank(self, exclude=(), only=None):
        if only is None:
            exclude = tuple(exclude) + tuple(getattr(self, 'bank_reserved', ()))
        while True:
            i = self.psi
            self.psi = (self.psi + 1) % 8
            if i not in exclude and (only is None or i in only):
                return i

    def load_consts(self):
        S = self.S
        self.ident = self.sb("ident", [128, 128], F32)
        S.dma('sp', lambda e: e.dma_start(out=self.ident[:], in_=self.cst['ident'][:, :]), w=['ident'])
        self.ones = self.sb("ones", [128, 128], F32)
        S.op('dve', lambda e: e.memset(self.ones[:], 1.0), w=['ones'])
        self.onesb = self.sb("onesb", [128, 128], BF16)
        S.op('dve', lambda e: e.memset(self.onesb[:], 1.0), w=['onesb'])
        self.identb = self.sb("identb", [128, 128], BF16)
        S.op('dve', lambda e: e.tensor_copy(out=self.identb[:], in_=self.ident[:]), r=['ident'], w=['identb'])

    def load_params(self):
        S = self.S
        for g in range(NPG):
            raw = self.sb("praw%d" % g, [128, 128], F32)
            S.dma('sp', lambda e, raw=raw, g=g: e.dma_start(out=raw[:], in_=self.pvec[g * 128:(g + 1) * 128, :]),
                  w=[('praw', g)])
            b = self.bank()
            S.op('pe', lambda e, raw=raw, b=b: e.transpose(out=self.ps[b][:, 0:128], in_=raw[:], identity=self.ident[:]),
                 r=[('praw', g), 'ident'], w=[('ps', b)])
            S.op('dve', lambda e, b=b, g=g: e.tensor_copy(out=self.PC[:, g * 128:(g + 1) * 128], in_=self.ps[b][:, 0:128]),
                 r=[('ps', b)], w=['PC'])

    def pc(self, name, i=0, n=None):
        o, k = PV_OFF[name]
        if n is None:
            n = k - i
        return self.PC[:, o + i:o + i + n]

    def ada_begin(self, units):
        self._ada_ring = [self.sb("adaw%d" % i, [128, KC, 128], F32) for i in range(3)]
        self._ada_ringb = [self.sb("adawb%d" % i, [128, KC, 128], BF16) for i in range(2)]
        if not hasattr(self, '_ada_sc'):
            raise RuntimeError
        self._ada_q = list(units)
        self._ada_cast_eng = 'pool'
        self._ada_dma_i = 0
        self._ada_mm_i = 0
        self._ada_base = getattr(self, '_ada_base', 0)

    def ada_step(self, n=1):
        S = self.S
        sc = self._ada_sc
        for _ in range(n):
            while self._ada_dma_i < len(self._ada_q) and self._ada_dma_i < self._ada_mm_i + 3:
                l, fj = self._ada_q[self._ada_dma_i]
                slot = (self._ada_base + self._ada_dma_i) % 3
                wt = self._ada_ring[self._ada_dma_i % 3]
                S.dma('sp', lambda e, wt=wt, l=l, fj=fj: e.dma_start(
                    out=wt[:], in_=self.ada_w[l, :, fj * 128:(fj + 1) * 128].rearrange("(k p) f -> p k f", p=128)),
                    w=[('adaw', self._ada_dma_i % 3)])
                self._ada_dma_i += 1
            if self._ada_mm_i >= len(self._ada_q):
                return
            l, fj = self._ada_q[self._ada_mm_i]
            wt = self._ada_ring[self._ada_mm_i % 3]
            wk = ('adaw', self._ada_mm_i % 3)
            wb = self._ada_ringb[self._ada_mm_i % 2]
            wbk = ('adawb', self._ada_mm_i % 2)
            self._ada_mm_i += 1
            if self._ada_mm_i % 3 == 0:
                S.op('act', lambda e, wt=wt, wb=wb: e.activation(out=wb[:], in_=wt[:], func=AF.Copy), r=[wk], w=[wbk])
            else:
                S.op('dve', lambda e, wt=wt, wb=wb: e.tensor_copy(out=wb[:], in_=wt[:]), r=[wk], w=[wbk])
            b = self.bank(only=self._ada_banks) if getattr(self, '_ada_banks', None) else self.bank()
            for kc in range(KC):
                S.op('pe', lambda e, wb=wb, kc=kc, b=b: e.matmul(self.ps[b][:, 0:2], lhsT=wb[:, kc, :], rhs=self._ada_scb[:, kc, :],
                                                               start=(kc == 0), stop=(kc == KC - 1)), r=[wbk, 'scb'], w=[('ps', b)])
            o, k = PV_OFF['ada_b%d' % l]
            S.op('dve', lambda e, l=l, fj=fj, b=b, o=o: e.tensor_tensor(
                out=self.M[l][:, fj, :], in0=self.ps[b][:, 0:2], in1=self.PC[:, o + fj:o + fj + 1].to_broadcast([128, 2]), op=ALU.add),
                r=[('ps', b), 'PC'], w=[('M', l, fj // KC)])

    def ada_flush(self):
        while self._ada_mm_i < len(self._ada_q):
            self.ada_step(1)

    def mcol(self, l, which, kc, ctx):
        return self.M[l][:, which * KC + kc, (1 if ctx else 0):(2 if ctx else 1)]

    def make_gs(self, l, nrm):
        S = self.S
        if not hasattr(self, 'GS'):
            self.GS = {}
        for ctx in range(2):
            g = self.GSbuf[(l, nrm, ctx)]
            sidx = 1 if nrm == 0 else 4
            gname = ('norm_mix%d' if nrm == 0 else 'norm_ffn%d') % l
            S.op('dve', lambda e, g=g, l=l, sidx=sidx, ctx=ctx, gname=gname: e.scalar_tensor_tensor(
                out=g[:], in0=self.M[l][:, sidx * KC:(sidx + 1) * KC, ctx], scalar=1.0, in1=self.pc(gname),
                op0=ALU.add, op1=ALU.mult), r=[('M', l, sidx), 'PC'], w=[('GS', l, nrm, ctx)])
            self.GS[(l, nrm, ctx)] = g

    def nmt(self, src, tiles, l, nrm, hT, h32=None, tag=""):
        S = self.S
        shidx = 0 if nrm == 0 else 3
        if not hasattr(self, '_nmt_bufs'):
            self._nmt_bufs = None
        xts = [self.sb("nmt_x%d" % i, [128, D], F32) for i in range(2)]
        junk = self.sb("nmt_junk", [128, D], BF16)
        st = self.sb("nmt_st", [128, 4 * NT], F32)
        for n, i in enumerate(tiles):
            ctx = 1 if i >= TL // 128 else 0
            xt = xts[n % 2]
            xk = ('nmt_x', n % 2)
            S.dma('sp', lambda e, xt=xt, i=i: e.dma_start(out=xt[:], in_=src[i * 128:(i + 1) * 128, :]),
                  r=[('xs', i)], w=[xk])
            ss = st[:, 4 * n:4 * n + 1]
            rs = st[:, 4 * n + 1:4 * n + 2]
            sk = ('nmt_st', n)
            S.op('act', lambda e, xt=xt, ss=ss: e.activation(out=junk[:], in_=xt[:], func=AF.Square, accum_out=ss),
                 r=[xk], w=[sk, 'nmt_junk'])
            S.op('act', lambda e, ss=ss: e.activation(out=ss, in_=ss, func=AF.Sqrt, bias=self.epsc[:, 0:1], scale=1.0 / D),
                 r=[sk, 'epsc'], w=[sk])
            S.op('dve', lambda e, ss=ss, rs=rs: e.reciprocal(out=rs, in_=ss), r=[sk], w=[sk])
            S.op('dve', lambda e, xt=xt, rs=rs: e.tensor_scalar(out=xt[:], in0=xt[:], scalar1=rs, scalar2=None, op0=ALU.mult),
                 r=[sk, xk], w=[xk])
            gs = self.GS[(l, nrm, ctx)]
            for q in range(4):
                b = self.bank()
                for j in range(4):
                    kc = q * 4 + j
                    S.op('pe', lambda e, xt=xt, b=b, j=j, kc=kc: e.transpose(
                        out=self.ps[b][:, j * 128:(j + 1) * 128], in_=xt[:, kc * 128:(kc + 1) * 128], identity=self.ident[:]),
                        r=[xk, 'ident'], w=[('ps', b)])
                for j in range(4):
                    kc = q * 4 + j
                    dst = hT[:, kc, i * 128:(i + 1) * 128] if h32 is None else h32[:, kc, :]
                    wk = ('hT', tag, i) if h32 is None else ('h32', tag)
                    eng = 'act' if (j % 2 == 0) else 'dve'
                    sh = self.mcol(l, shidx, kc, ctx)
                    if eng == 'act':
                        S.op('act', lambda e, dst=dst, b=b, j=j, kc=kc, gs=gs, sh=sh: e.activation(
                            out=dst, in_=self.ps[b][:, j * 128:(j + 1) * 128], func=AF.Identity, bias=sh, scale=gs[:, kc:kc + 1]),
                            r=[('ps', b), ('GS', l, nrm, ctx), ('M', l, shidx)], w=[wk])
                    else:
                        S.op('dve', lambda e, dst=dst, b=b, j=j, kc=kc, gs=gs, sh=sh: e.tensor_scalar(
                            out=dst, in0=self.ps[b][:, j * 128:(j + 1) * 128], scalar1=gs[:, kc:kc + 1], scalar2=sh,
                            op0=ALU.mult, op1=ALU.add),
                            r=[('ps', b), ('GS', l, nrm, ctx), ('M', l, shidx)], w=[wk])
            if h32 is not None:
                S.op('pool', lambda e, i=i: e.tensor_copy(out=hT[:, :, i * 128:(i + 1) * 128], in_=h32[:, :, :]),
                     r=[('h32', tag)], w=[('hT', tag, i)])

    def tblocks(self, t0, t1):
        out = []
        t = t0
        while t < t1:
            n = min(512, t1 - t)
            out.append((t, n))
            t += n
        return out

    def linear_fm(self, w, groups, hT, tag, evac, done, t1=T):
        S = self.S
        slabs = [self.sb("wslab%d" % i, [128, KC, 512], BF16) for i in range(2)]
        for gi, grp in enumerate(groups):
            sl = slabs[gi % 2]
            sk = ('wslab', gi % 2)
            c0 = grp[0] * 128
            ncol = len(grp) * 128
            for half in range(2):
                S.dma('pool', lambda e, sl=sl, c0=c0, ncol=ncol, half=half: e.dma_start(
                    out=sl[:, half * 8:(half + 1) * 8, 0:ncol],
                    in_=w[half * 1024:(half + 1) * 1024, c0:c0 + ncol].rearrange("(k p) f -> p k f", p=128)), w=[sk])
            for jj, g in enumerate(grp):
                for (t0, n) in self.tblocks(0, t1):
                    b = self.bank()
                    for kc in range(KC):
                        S.op('pe', lambda e, sl=sl, jj=jj, kc=kc, b=b, t0=t0, n=n: e.matmul(
                            self.ps[b][:, 0:n], lhsT=sl[:, kc, jj * 128:(jj + 1) * 128], rhs=hT[:, kc, t0:t0 + n],
                            start=(kc == 0), stop=(kc == KC - 1)),
                            r=[sk] + [('hT', tag, i) for i in range(t0 // 128, (t0 + n) // 128)], w=[('ps', b)])
                    evac(g, t0, n, self.ps[b][:, 0:n], b)
                done(g)

    def linear_tm(self, w, colblocks, hT, tag, tiles, evac):
        S = self.S
        slabs = [self.sb("wslabt%d" % i, [128, KC, 512], BF16) for i in range(2)]
        for gi, c0 in enumerate(colblocks):
            sl = slabs[gi % 2]
            sk = ('wslabt', gi % 2)
            for half in range(2):
                S.dma('pool', lambda e, sl=sl, c0=c0, half=half: e.dma_start(
                    out=sl[:, half * 8:(half + 1) * 8, :],
                    in_=w[half * 1024:(half + 1) * 1024, c0:c0 + 512].rearrange("(k p) f -> p k f", p=128)), w=[sk])
            for i in tiles:
                b = self.bank()
                for kc in range(KC):
                    S.op('pe', lambda e, sl=sl, kc=kc, b=b, i=i: e.matmul(
                        self.ps[b][:, :], lhsT=hT[:, kc, i * 128:(i + 1) * 128], rhs=sl[:, kc, :],
                        start=(kc == 0), stop=(kc == KC - 1)),
                        r=[sk, ('hT', tag, i)], w=[('ps', b)])
                evac(gi, c0, i, self.ps[b][:, :], b)

    def phase_prep(self):
        S = self.S
        self.load_consts()
        self.epsc = self.sb("epsc", [128, 1], F32)
        S.op('dve', lambda e: e.memset(self.epsc[:], EPS), w=['epsc'])
        self.PC = self.sb("PC", [128, PV_ROWS], F32)
        self.M = [self.sb("M%d" % l, [128, 96, 2], F32) for l in range(2)]
        self.GSbuf = {(l, nrm, ctx): self.sb("gs%d%d%d" % (l, nrm, ctx), [128, KC], F32)
                      for l in range(2) for nrm in range(2) for ctx in range(2)}
        self.LB = self.sb("LB", [128, 16], F32)
        self.OML = self.sb("OML", [128, 16], F32)
        self.NEGLAM = self.sb("NEGLAM", [128, 1], F32)
        self._ada_sc = self.sb("silu_c", [128, KC, 2], F32)
        self._ada_scb = self.sb("silu_cb", [128, KC, 2], BF16)
        self.nkeep = len(self.live)
        self.load_params()
        S.op('act', lambda e: e.activation(out=self._ada_sc[:, :, 0], in_=self.pc('c'), func=AF.Silu), r=['PC'], w=['sc'])
        S.op('act', lambda e: e.activation(out=self._ada_sc[:, :, 1], in_=self.pc('c_ctx'), func=AF.Silu), r=['PC'], w=['sc'])
        S.op('dve', lambda e: e.tensor_copy(out=self._ada_scb[:], in_=self._ada_sc[:]), r=['sc'], w=['scb'])
        self.make_lam(0.8 - 0.6 * math.exp(-0.3 * 0))
        for d_, (n0, n1) in enumerate((('lbl_f0', 'lbl_f1'), ('lbl_b0', 'lbl_b1'))):
            S.op('dve', lambda e, d_=d_, n0=n0, n1=n1: e.tensor_tensor(out=self.LB[:, d_ * 8:(d_ + 1) * 8], in0=self.pc(n0),
                                                                  in1=self.pc(n1), op=ALU.subtract), r=['PC'], w=['LB'])
        S.op('act', lambda e: e.activation(out=self.LB[:], in_=self.LB[:], func=AF.Sigmoid), r=['LB'], w=['LB'])
        S.op('dve', lambda e: e.tensor_scalar(out=self.OML[:], in0=self.LB[:], scalar1=-1.0, scalar2=1.0, op0=ALU.mult, op1=ALU.add),
             r=['LB'], w=['OML'])
        self.ada_begin([(0, fj) for fj in range(2 * KC)])
        self.ada_flush()
        self.make_gs(0, 0)
        self.free_phase(self.nkeep)

    def phase_inproj0(self):
        S = self.S
        hT = self.sb("hT", [128, KC, T], BF16)
        self.nmt(self.xin, list(range(NT)), 0, 0, hT, tag="a")
        stg = [self.sb("stg%d" % i, [128, T], F32) for i in range(2)]
        cnt = [0]

        def evac(g, t0, n, pap, b):
            st = stg[cnt[0] % 2]
            fam = g // 8
            func = AF.Silu if fam in (0, 4) else AF.Copy
            S.op('act', lambda e, st=st, t0=t0, n=n, pap=pap, func=func: e.activation(out=st[:, t0:t0 + n], in_=pap, func=func),
                 r=[('ps', b)], w=[('stg', cnt[0] % 2)])

        def done(g):
            st = stg[cnt[0] % 2]
            S.dma('sp', lambda e, st=st, g=g: e.dma_start(out=self.projT[g * 128:(g + 1) * 128, :], in_=st[:]),
                  r=[('stg', cnt[0] % 2)], w=[('projT', g)])
            cnt[0] += 1

        fm_groups = []
        for fam in (0, 1, 2, 4, 5, 6):
            fm_groups.append([fam * 8 + j for j in range(4)])
            fm_groups.append([fam * 8 + 4 + j for j in range(4)])
        self.linear_fm(self.w_in0, fm_groups, hT, "a", evac, done)
        vst = [self.sb("vst%d" % i, [128, 512], BF16) for i in range(2)]
        vc = [0]

        def evac_v(gi, c0, i, pap, b):
            st = vst[vc[0] % 2]
            k = ('vst', vc[0] % 2)
            h0 = gi * 4
            S.op('act', lambda e, st=st, pap=pap: e.activation(out=st[:], in_=pap, func=AF.Copy), r=[('ps', b)], w=[k])
            S.dma('sp', lambda e, st=st, h0=h0, i=i: e.dma_start(
                out=self.vtok[h0:h0 + 4, i * 128:(i + 1) * 128, :].rearrange("h t c -> t h c"),
                in_=st[:].rearrange("t (h c) -> t h c", c=128)), r=[k], w=[('vtok', h0, i)])
            vc[0] += 1

        self.linear_tm(self.w_in0, [3072, 3584, 7168, 7680], hT, "a", list(range(NT)), evac_v)
        self.free_phase(self.nkeep)


    def rms_gate_store(self, oT, gate_col, post_scale, sg, row0, tcols, pfx):
        S = self.S
        sq = self.sb(pfx + "sq", [128, T], BF16)
        rstd = self.sb(pfx + "rstd", [128, T], F32)
        ob = self.sb(pfx + "ob", [128, T], BF16)
        S.op('act', lambda e: e.activation(out=sq[:, 0:tcols], in_=oT[:, 0:tcols], func=AF.Square), r=[pfx + 'oT'], w=[pfx + 'sq'])
        for (t0, n) in self.tblocks(0, tcols):
            b = self.bank()
            S.op('pe', lambda e, b=b, t0=t0, n=n: e.matmul(self.ps[b][:, 0:n], lhsT=self.onesb[:], rhs=sq[:, t0:t0 + n],
                                                          start=True, stop=True), r=[pfx + 'sq', 'onesb'], w=[('ps', b)])
            S.op('act', lambda e, b=b, t0=t0, n=n: e.activation(out=rstd[:, t0:t0 + n], in_=self.ps[b][:, 0:n], func=AF.Sqrt,
                                                               bias=self.epsc[:, 0:1], scale=1.0 / 128.0),
                 r=[('ps', b), 'epsc'], w=[pfx + 'rstd'])
        S.op('dve', lambda e: e.reciprocal(out=rstd[:, 0:tcols], in_=rstd[:, 0:tcols]), r=[pfx + 'rstd'], w=[pfx + 'rstd'])
        S.op('dve', lambda e: e.tensor_tensor(out=rstd[:, 0:tcols], in0=rstd[:, 0:tcols], in1=oT[:, 0:tcols], op=ALU.mult),
             r=[pfx + 'rstd', pfx + 'oT'], w=[pfx + 'rstd'])
        if sg is not None:
            S.op('dve', lambda e: e.scalar_tensor_tensor(out=ob[:, 0:tcols], in0=rstd[:, 0:tcols], scalar=gate_col, in1=sg[:, 0:tcols],
                                                        op0=ALU.mult, op1=ALU.mult), r=[pfx + 'rstd', pfx + 'sg', 'PC'], w=[pfx + 'ob'])
        else:
            S.op('dve', lambda e: e.tensor_scalar(out=ob[:, 0:tcols], in0=rstd[:, 0:tcols], scalar1=gate_col, scalar2=post_scale,
                                                 op0=ALU.mult, op1=ALU.mult), r=[pfx + 'rstd', 'PC'], w=[pfx + 'ob'])
        return ob

    def phase_hgrn(self):
        S = self.S
        self.ada_begin([(0, fj) for fj in range(2 * KC, 6 * KC)] + [(1, fj) for fj in range(6 * KC)])
        m01 = self.sb("m01", [128, T + 1], F32)
        S.dma('sp', lambda e: e.dma_start(out=m01[:], in_=self.cst['m01'][:, :]), w=['m01'])
        masks = []
        for nm in ('maskf', 'maskb'):
            mk = self.sb(nm, [128, 128], F32)
            S.dma('sp', lambda e, mk=mk, nm=nm: e.dma_start(out=mk[:], in_=self.cst[nm][:, :]), w=[nm])
            masks.append(mk)
        qT = self.sb("hq", [128, T], F32)
        fl = [self.sb("hf%d" % d_, [128, T], F32) for d_ in range(2)]
        sgs = [self.sb("hg%d" % i, [128, T], F32) for i in range(2)]
        vtms = [self.sb("hv%d" % i, [128, NT, 128], BF16) for i in range(2)]
        kT = self.sb("hk", [128, T], F32)
        cum = self.sb("hcum", [128, T], F32)
        E = self.sb("hE", [128, T], F32)
        k2f = self.sb("hk2f", [128, T], F32)
        oT = k2f
        qt = [self.sb("hqt%d" % d_, [128, T], BF16) for d_ in range(2)]
        kt = [self.sb("hkt%d" % d_, [128, T], BF16) for d_ in range(2)]
        k2t = [self.sb("hk2t%d" % d_, [128, NT, 128], BF16) for d_ in range(2)]
        Sbf = [self.sb("hSbf%d" % d_, [128, NCHK, 128], BF16) for d_ in range(2)]
        dec = [self.sb("hdec%d" % d_, [128, NCHK], F32) for d_ in range(2)]
        Sst = [[self.sb("hS%d_%d" % (d_, i), [128, 128], F32) for i in range(2)] for d_ in range(2)]
        attm = [self.sb("hattm%d" % i, [128, 128], BF16) for i in range(4)]
        nlat = TL // CH

        def load_qf(h):
            S.dma('sp', lambda e: e.dma_start(out=qT[:], in_=self.projT[h * 128:(h + 1) * 128, :]), r=[('projT', h)], w=['hq'])
            for d_ in range(2):
                S.dma('sp', lambda e, d_=d_: e.dma_start(out=fl[d_][:], in_=self.projT[(8 + d_ * 8 + h) * 128:(9 + d_ * 8 + h) * 128, :]),
                      r=[('projT', 8 + d_ * 8 + h)], w=[('hf', d_)])

        def load_gv(h):
            S.dma('sp', lambda e: e.dma_start(out=sgs[h % 2][:], in_=self.projT[(32 + h) * 128:(33 + h) * 128, :]), r=[('projT', 32 + h)], w=[('hsg', h % 2)])
            S.dma('sp', lambda e: e.dma_start(out=vtms[h % 2][:], in_=self.vtok[h, :, :].rearrange("(n p) c -> p n c", p=128)),
                  r=[('vtok', (h // 4) * 4, i) for i in range(NT)], w=[('hv', h % 2)])

        load_qf(0)
        load_gv(0)
        for h in range(8):
            sg = sgs[h % 2]
            vtm = vtms[h % 2]
            vk = ('hv', h % 2)
            if h + 1 < 8:
                load_gv(h + 1)
            for d_ in range(2):
                f = fl[d_]
                fk = ('hf', d_)
                lbc = self.LB[:, d_ * 8 + h:d_ * 8 + h + 1]
                omc = self.OML[:, d_ * 8 + h:d_ * 8 + h + 1]
                S.op('act', lambda e, f=f: e.activation(out=f[:], in_=f[:], func=AF.Sigmoid), r=[fk], w=[fk])
                S.op('dve', lambda e, f=f, lbc=lbc, omc=omc: e.tensor_scalar(out=f[:], in0=f[:], scalar1=omc, scalar2=lbc,
                                                                           op0=ALU.mult, op1=ALU.add), r=[fk, 'LB', 'OML'], w=[fk])
                S.op('pool', lambda e, f=f: e.tensor_scalar(out=kT[:], in0=f[:], scalar1=-1.0, scalar2=1.0, op0=ALU.mult, op1=ALU.add),
                     r=[fk], w=['hk'])
                S.op('act', lambda e, f=f: e.activation(out=f[:], in_=f[:], func=AF.Ln), r=[fk, 'hk'], w=[fk])
                if d_ == 0:
                    S.op('dve', lambda e, f=f: e.tensor_tensor_scan(out=cum[:, 0:T], data0=m01[:, 0:T], data1=f[:, 0:T], initial=0.0,
                                                                   op0=ALU.mult, op1=ALU.add), r=[fk, 'm01'], w=['hcum'])
                else:
                    S.op('dve', lambda e, f=f: e.tensor_tensor_scan(out=cum[:, ::-1], data0=m01[:, 1:T + 1][:, ::-1], data1=f[:, ::-1],
                                                                   initial=0.0, op0=ALU.mult, op1=ALU.add), r=[fk, 'm01'], w=['hcum'])
                S.op('act', lambda e: e.activation(out=E[:], in_=cum[:], func=AF.Exp), r=['hcum'], w=['hE'])
                S.op('dve', lambda e, d_=d_: e.tensor_tensor(out=qt[d_][:], in0=qT[:], in1=E[:], op=ALU.mult), r=['hq', 'hE'], w=[('hqt', d_)])
                S.op('act', lambda e: e.activation(out=E[:], in_=cum[:], func=AF.Exp, scale=-1.0), r=['hcum', ('hqt', d_)], w=['hE'])
                S.op('dve', lambda e, d_=d_: e.tensor_tensor(out=kt[d_][:], in0=kT[:], in1=E[:], op=ALU.mult), r=['hk', 'hE'], w=[('hkt', d_)])
                cum3 = cum[:].rearrange("p (c j) -> p c j", j=CH)
                lastj = (CH - 1) if d_ == 0 else 0
                S.op('act', lambda e, d_=d_, cum3=cum3, lastj=lastj: e.activation(out=dec[d_][:], in_=cum3[:, :, lastj], func=AF.Exp),
                     r=['hcum'], w=[('hdec', d_)])
                E3 = E[:].rearrange("p (c j) -> p c j", j=CH)
                S.op('dve', lambda e, cum3=cum3, E3=E3, lastj=lastj: e.tensor_tensor(
                    out=E3, in0=cum3[:, :, lastj:lastj + 1].to_broadcast([128, NCHK, CH]), in1=cum3, op=ALU.subtract),
                    r=['hcum', ('hkt', d_)], w=['hE'])
                S.op('act', lambda e: e.activation(out=E[:], in_=E[:], func=AF.Exp), r=['hE'], w=['hE'])
                S.op('dve', lambda e: e.tensor_tensor(out=k2f[:], in0=kT[:], in1=E[:], op=ALU.mult), r=['hk', 'hE'], w=['hk2f'])
                for q in range((NT + 3) // 4):
                    b = self.bank()
                    nt_ = min(4, NT - q * 4)
                    for j in range(nt_):
                        i = q * 4 + j
                        S.op('pe', lambda e, b=b, j=j, i=i: e.transpose(out=self.ps[b][:, j * 128:(j + 1) * 128],
                                                                        in_=k2f[:, i * 128:(i + 1) * 128], identity=self.ident[:]),
                             r=['hk2f', 'ident'], w=[('ps', b)])
                    S.op('act', lambda e, b=b, q=q, nt_=nt_, d_=d_: e.activation(
                        out=k2t[d_][:, q * 4:q * 4 + nt_, :], in_=self.ps[b][:, 0:nt_ * 128].rearrange("p (n c) -> p n c", c=128),
                        func=AF.Copy), r=[('ps', b)], w=[('hk2t', d_)])
            if h + 1 < 8:
                load_qf(h + 1)
            orders = [list(range(nlat, NCHK)) + list(range(0, nlat)),
                      list(range(NCHK - 1, nlat - 1, -1)) + list(range(nlat - 1, -1, -1))]
            cb_ = [None, None]
            for n in range(NCHK):
                if n % 2 == 0:
                    self.ada_step(1)
                for d_ in range(2):
                    c = orders[d_][n]
                    if n % 4 == 0:
                        cb_[d_] = self.bank()
                    b = cb_[d_]
                    i, half = c // 2, c % 2
                    pa = self.ps[b][:, (n % 4) * 128:(n % 4 + 1) * 128]
                    S.op('pe', lambda e, pa=pa, i=i, half=half, d_=d_, vtm=vtm: e.matmul(
                        pa, lhsT=k2t[d_][half * 64:(half + 1) * 64, i, :], rhs=vtm[half * 64:(half + 1) * 64, i, :], start=True, stop=True),
                        r=[('hk2t', d_), vk], w=[('ps', b)])
                    Sn = Sst[d_][n % 2]
                    So = Sst[d_][(n + 1) % 2]
                    if n == 0:
                        S.op('dve', lambda e, Sn=Sn, pa=pa: e.tensor_copy(out=Sn[:], in_=pa), r=[('ps', b)], w=[('hS', d_, n % 2)])
                    else:
                        S.op('dve', lambda e, Sn=Sn, So=So, pa=pa, c=c, d_=d_: e.scalar_tensor_tensor(
                            out=Sn[:], in0=So[:], scalar=dec[d_][:, c:c + 1], in1=pa, op0=ALU.mult, op1=ALU.add),
                            r=[('ps', b), ('hS', d_, (n + 1) % 2), ('hdec', d_)], w=[('hS', d_, n % 2)])
                    if n + 1 < NCHK:
                        cn = orders[d_][n + 1]
                        S.op('act', lambda e, Sn=Sn, cn=cn, d_=d_: e.activation(out=Sbf[d_][:, cn, :], in_=Sn[:], func=AF.Copy),
                             r=[('hS', d_, n % 2)], w=[('hSbf', d_, cn)])
            first = [nlat, NCHK - 1]

            def emit_att(i):
                for d_ in range(2):
                    ba = 4 + ((2 * i + d_) % 4)
                    am = attm[(2 * i + d_) % 4]
                    S.op('pe', lambda e, ba=ba, i=i, d_=d_: e.matmul(self.ps[ba][:, 0:128], lhsT=kt[d_][:, i * 128:(i + 1) * 128],
                                                                     rhs=qt[d_][:, i * 128:(i + 1) * 128], start=True, stop=True),
                         r=[('hkt', d_), ('hqt', d_)], w=[('ps', ba)])
                    S.op('dve', lambda e, ba=ba, am=am, d_=d_: e.tensor_tensor(out=am[:], in0=self.ps[ba][:, 0:128], in1=masks[d_][:], op=ALU.mult),
                         r=[('ps', ba), 'maskf', 'maskb'], w=[('hattm', (2 * i + d_) % 4)])

            emit_att(0)
            for i in range(NT):
                if i + 1 < NT:
                    emit_att(i + 1)
                q, j = i // 4, i % 4
                bo = q % 4
                po = self.ps[bo][:, j * 128:(j + 1) * 128]
                mms = []
                for d_ in range(2):
                    mms.append(('pv', d_, None, None))
                    for half in range(2):
                        c = 2 * i + half
                        if c != first[d_]:
                            mms.append(('s', d_, c, half))
                for mi, (kind, d_, c, half) in enumerate(mms):
                    st_, sp_ = (mi == 0), (mi == len(mms) - 1)
                    if kind == 'pv':
                        am = attm[(2 * i + d_) % 4]
                        S.op('pe', lambda e, po=po, i=i, am=am, st_=st_, sp_=sp_, vtm=vtm: e.matmul(po, lhsT=vtm[:, i, :], rhs=am[:], start=st_, stop=sp_),
                             r=[vk, ('hattm', (2 * i + d_) % 4)], w=[('ps', bo)])
                    else:
                        S.op('pe', lambda e, po=po, c=c, half=half, d_=d_, st_=st_, sp_=sp_: e.matmul(
                            po[:, half * 64:(half + 1) * 64], lhsT=Sbf[d_][:, c, :], rhs=qt[d_][:, c * 64:(c + 1) * 64], start=st_, stop=sp_),
                            r=[('hSbf', d_, c), ('hqt', d_)], w=[('ps', bo)])
                if j == 3 or i == NT - 1:
                    nt_ = j + 1
                    S.op('act', lambda e, bo=bo, q=q, nt_=nt_: e.activation(out=oT[:, q * 512:q * 512 + nt_ * 128], in_=self.ps[bo][:, 0:nt_ * 128], func=AF.Copy),
                         r=[('ps', bo)], w=['hk2f'])
            self.ada_step(2)
            ob = self._hg_out(oT, sg, h, okey='hk2f', sgkey=('hsg', h % 2))
        self.ada_flush()
        self.make_gs(0, 1)
        self.make_gs(1, 0)
        self.make_gs(1, 1)
        self.free_phase(self.nkeep)

    def make_lam(self, lam_init):
        S = self.S
        hm = self.sb("halfm", [128, 2], F32)
        S.dma('sp', lambda e: e.dma_start(out=hm[:], in_=self.cst['halfm'][:, :]), w=['halfm'])
        pr = self.sb("lamp", [128, 4], F32)
        S.op('dve', lambda e: e.tensor_tensor(out=pr[:, 0:1], in0=self.pc('dl_a'), in1=self.pc('dl_b'), op=ALU.mult), r=['PC'], w=['lamp'])
        S.op('dve', lambda e: e.tensor_scalar(out=pr[:, 2:4], in0=hm[:], scalar1=pr[:, 0:1], scalar2=None, op0=ALU.mult), r=['lamp', 'halfm'], w=['lamp2'])
        b = self.bank()
        S.op('pe', lambda e: e.matmul(self.ps[b][:, 0:2], lhsT=self.ones[:], rhs=pr[:, 2:4], start=True, stop=True), r=['lamp2', 'ones'], w=[('ps', b)])
        S.op('act', lambda e: e.activation(out=pr[:, 2:4], in_=self.ps[b][:, 0:2], func=AF.Exp), r=[('ps', b)], w=['lamp2'])
        S.op('dve', lambda e: e.tensor_tensor(out=pr[:, 0:1], in0=pr[:, 3:4], in1=pr[:, 2:3], op=ALU.subtract), r=['lamp2'], w=['lamp'])
        S.op('dve', lambda e: e.tensor_scalar(out=self.NEGLAM[:], in0=pr[:, 0:1], scalar1=-lam_init, scalar2=None, op0=ALU.add), r=['lamp'], w=['NEGLAM'])

    def _hg_out(self, oT, sg, h, gain=None, post=1.0, okey='h_oT', sgkey='hsg'):
        S = self.S
        if not hasattr(self, '_hgbufs'):
            self._hgbufs = (self.sb("h_sq", [128, T], BF16), self.sb("h_rstd", [128, T], F32), self.sb("h_ob", [128, T], BF16))
        sq, rstd, ob = self._hgbufs
        S.op('act', lambda e: e.activation(out=sq[:], in_=oT[:], func=AF.Square), r=[okey], w=['h_sq'])
        for (t0, n) in self.tblocks(0, T):
            b = self.bank()
            S.op('pe', lambda e, b=b, t0=t0, n=n: e.matmul(self.ps[b][:, 0:n], lhsT=self.onesb[:], rhs=sq[:, t0:t0 + n], start=True, stop=True),
                 r=['h_sq', 'onesb'], w=[('ps', b)])
            S.op('act', lambda e, b=b, t0=t0, n=n: e.activation(out=rstd[:, t0:t0 + n], in_=self.ps[b][:, 0:n], func=AF.Ln,
                                                               bias=self.epsc[:, 0:1], scale=1.0 / 128.0), r=[('ps', b), 'epsc'], w=['h_rstd'])
        S.op('act', lambda e: e.activation(out=rstd[:], in_=rstd[:], func=AF.Exp, scale=-0.5), r=['h_rstd'], w=['h_rstd'])
        S.op('dve', lambda e: e.tensor_tensor(out=rstd[:], in0=rstd[:], in1=oT[:], op=ALU.mult), r=['h_rstd', okey], w=['h_rstd'])
        if sg is not None:
            S.op('dve', lambda e: e.scalar_tensor_tensor(out=ob[:], in0=rstd[:], scalar=self.pc('hgrn_gain'), in1=sg[:], op0=ALU.mult, op1=ALU.mult),
                 r=['h_rstd', sgkey, 'PC'], w=['h_ob'])
        else:
            S.op('dve', lambda e: e.tensor_scalar(out=ob[:], in0=rstd[:], scalar1=gain, scalar2=post, op0=ALU.mult, op1=ALU.mult),
                 r=['h_rstd', 'PC'], w=['h_ob'])
        S.dma('sp', lambda e, h=h: e.dma_start(out=self.mixT[h * 128:(h + 1) * 128, :], in_=ob[:]), r=['h_ob'], w=[('mixT', h)])
        return ob

    def phase_attn(self):
        S = self.S
        if hasattr(self, '_hgbufs'):
            del self._hgbufs
        MISC = (6, 7)
        ATT_DUMMY = False
        dummy_rhs = self.sb("a_dummy", [128, 512], BF16)
        S.op('dve', lambda e: e.memset(dummy_rhs[:], 0.001), w=['a_dummy'])

        pcg = self.precast_gen(0)
        ropec = self.sb("ropec", [128, TL], F32)
        ropes = self.sb("ropes", [128, TL], F32)
        S.dma('sp', lambda e: e.dma_start(out=ropec[:], in_=self.cst['ropec'][:, :]), w=['ropec'])
        S.dma('sp', lambda e: e.dma_start(out=ropes[:], in_=self.cst['ropes'][:, :]), w=['ropes'])
        tmpc = self.sb("a_tmpc", [128, 128], F32)
        rotb = self.sb("rotb", [128, 128], BF16)
        blkb = self.sb("blkb", [128, 128], BF16)
        for nm, dst in (('rotm', rotb), ('blk1', blkb)):
            S.dma('sp', lambda e, nm=nm: e.dma_start(out=tmpc[:], in_=self.cst[nm][:, :]), w=['a_tmpc'])
            S.op('dve', lambda e, dst=dst: e.tensor_copy(out=dst[:], in_=tmpc[:]), r=['a_tmpc'], w=['a_' + nm])
        X = self.sb("aX", [128, T], F32)
        sq = self.sb("asq", [128, T], BF16)
        rstd = self.sb("arstd", [128, T], F32)
        xnb = self.sb("axnb", [128, TL], BF16)
        t1 = self.sb("at1", [128, 512], F32)
        t2 = self.sb("at2", [128, 512], F32)
        qk = [[self.sb("aqk%d_%d" % (sl, i), [128, T], BF16) for i in range(2)] for sl in range(2)]
        vtm = [self.sb("av%d" % sl, [128, NT, 128], BF16) for sl in range(2)]
        ET = [self.sb("aET%d" % i, [128, 512], BF16) for i in range(4)]
        acc = [self.sb("aacc%d" % i, [128, 512], F32) for i in range(2)]
        tm = [self.sb("atm%d" % i, [128, 512], F32) for i in range(2)]
        rz = [self.sb("arz%d" % i, [128, 512], F32) for i in range(2)]
        aT = [self.sb("aaT%d" % sl, [128, T], F32) for sl in range(2)]
        osq = self.sb("aosq", [128, T], BF16)
        orstd = self.sb("aorstd", [128, T], F32)
        oob = self.sb("aoob", [128, T], BF16)
        post = 1.0 - (0.8 - 0.6 * math.exp(-0.3 * 0))

        def prep_gen(h):
            sl = h % 2
            S.dma('sp', lambda e: e.dma_start(out=vtm[sl][:], in_=self.vtok[8 + h, :, :].rearrange("(n p) c -> p n c", p=128)),
                  r=[('vtok', 8 + (h // 4) * 4, i) for i in range(NT)], w=[('av', sl)])
            for which in range(2):
                row = (40 + which * 8 + h)
                xb = qk[sl][which]
                xk = ('aqk', sl, which)
                gcol = self.pc('gq' if which == 0 else 'gk')
                S.dma('sp', lambda e, row=row: e.dma_start(out=X[:], in_=self.projT[row * 128:(row + 1) * 128, :]), r=[('projT', row)], w=['aX'])
                S.op('act', lambda e: e.activation(out=sq[:], in_=X[:], func=AF.Square), r=['aX'], w=['asq'])
                yield
                for (t0, n) in self.tblocks(0, T):
                    b = self.bank(only=MISC)
                    S.op('pe', lambda e, b=b, t0=t0, n=n: e.matmul(self.ps[b][:, 0:n], lhsT=blkb[:], rhs=sq[:, t0:t0 + n], start=True, stop=True),
                         r=['asq', 'a_blk1'], w=[('ps', b)])
                    S.op('act', lambda e, b=b, t0=t0, n=n: e.activation(out=rstd[:, t0:t0 + n], in_=self.ps[b][:, 0:n], func=AF.Ln,
                                                                       bias=self.epsc[:, 0:1], scale=1.0 / 64.0), r=[('ps', b), 'epsc'], w=['arstd'])
                    yield
                S.op('act', lambda e: e.activation(out=rstd[:], in_=rstd[:], func=AF.Exp, scale=-0.5), r=['arstd'], w=['arstd'])
                yield
                S.op('dve', lambda e, gcol=gcol: e.scalar_tensor_tensor(out=X[:], in0=X[:], scalar=gcol, in1=rstd[:], op0=ALU.mult, op1=ALU.mult),
                     r=['aX', 'arstd', 'PC'], w=['aX'])
                yield
                S.op('pool', lambda e: e.tensor_copy(out=xnb[:], in_=X[:, 0:TL]), r=['aX'], w=['axnb'])
                S.op('pool', lambda e, xb=xb: e.tensor_copy(out=xb[:, TL:T], in_=X[:, TL:T]), r=['aX'], w=[xk])
                yield
                for (t0, n) in self.tblocks(0, TL):
                    b = self.bank(only=MISC)
                    S.op('pe', lambda e, b=b, t0=t0, n=n: e.matmul(self.ps[b][:, 0:n], lhsT=rotb[:], rhs=xnb[:, t0:t0 + n], start=True, stop=True),
                         r=['axnb', 'a_rotm'], w=[('ps', b)])
                    S.op('pool', lambda e, t0=t0, n=n: e.tensor_tensor(out=t2[:, 0:n], in0=X[:, t0:t0 + n], in1=ropec[:, t0:t0 + n], op=ALU.mult),
                         r=['aX', 'ropec'], w=['at2'])
                    S.op('dve', lambda e, b=b, t0=t0, n=n: e.tensor_tensor(out=t1[:, 0:n], in0=self.ps[b][:, 0:n], in1=ropes[:, t0:t0 + n], op=ALU.mult),
                         r=[('ps', b), 'ropes'], w=['at1'])
                    S.op('pool', lambda e, xb=xb, t0=t0, n=n: e.tensor_tensor(out=xb[:, t0:t0 + n], in0=t1[:, 0:n], in1=t2[:, 0:n], op=ALU.add),
                         r=['at1', 'at2'], w=[xk])
                    yield

        def out_gen(h):
            sl = h % 2
            a_ = aT[sl]
            ak = ('aaT', sl)
            S.op('act', lambda e: e.activation(out=osq[:], in_=a_[:], func=AF.Square), r=[ak], w=['aosq'])
            yield
            for (t0, n) in self.tblocks(0, T):
                b = self.bank(only=MISC)
                S.op('pe', lambda e, b=b, t0=t0, n=n: e.matmul(self.ps[b][:, 0:n], lhsT=self.onesb[:], rhs=osq[:, t0:t0 + n], start=True, stop=True),
                     r=['aosq', 'onesb'], w=[('ps', b)])
                S.op('act', lambda e, b=b, t0=t0, n=n: e.activation(out=orstd[:, t0:t0 + n], in_=self.ps[b][:, 0:n], func=AF.Ln,
                                                                   bias=self.epsc[:, 0:1], scale=1.0 / 128.0), r=[('ps', b), 'epsc'], w=['aorstd'])
                yield
            S.op('act', lambda e: e.activation(out=orstd[:], in_=orstd[:], func=AF.Exp, scale=-0.5), r=['aorstd'], w=['aorstd'])
            yield
            S.op('pool', lambda e: e.tensor_tensor(out=orstd[:], in0=orstd[:], in1=a_[:], op=ALU.mult), r=['aorstd', ak], w=['aorstd'])
            yield
            S.op('pool', lambda e: e.tensor_scalar(out=oob[:], in0=orstd[:], scalar1=self.pc('diff_gain'), scalar2=post, op0=ALU.mult, op1=ALU.mult),
                 r=['aorstd', 'PC'], w=['aoob'])
            S.dma('sp', lambda e: e.dma_start(out=self.mixT[(8 + h) * 128:(9 + h) * 128, :], in_=oob[:]), r=['aoob'], w=[('mixT', 8 + h)])
            yield

        etc = [0]

        def main_gen(h):
            sl = h % 2
            qb, kb = qk[sl]
            vt = vtm[sl]
            a_ = aT[sl]
            qblocks = [(t0, n, list(range(NT))) for (t0, n) in self.tblocks(0, TL)] + [(TL, TCX, list(range(TL // 128, NT)))]
            its = []
            for (t0, n, ktiles) in qblocks:
                for ki, kt_ in enumerate(ktiles):
                    its.append(dict(t0=t0, n=n, ki=ki, kt=kt_, nk=len(ktiles)))
            for idx, it in enumerate(its):
                it['idx'] = idx

            def emit_qk(it):
                kt_, t0, n = it['kt'], it['t0'], it['n']
                for m in range(2):
                    bs = 2 + 2 * (it['idx'] % 2) + m
                    S.op('pe', lambda e, bs=bs, m=m, kt_=kt_, t0=t0, n=n: e.matmul(
                        self.ps[bs][:, 0:n], lhsT=kb[m * 64:(m + 1) * 64, kt_ * 128:(kt_ + 1) * 128], rhs=qb[m * 64:(m + 1) * 64, t0:t0 + n],
                        start=True, stop=True), r=[('aqk', sl, 0), ('aqk', sl, 1)], w=[('ps', bs)])

            emit_qk(its[0])
            for idx, it in enumerate(its):
                if idx + 1 < len(its):
                    emit_qk(its[idx + 1])
                t0, n, ki, kt_, nk = it['t0'], it['n'], it['ki'], it['kt'], it['nk']
                ets = []
                for m in range(2):
                    bs = 2 + 2 * (idx % 2) + m
                    et = ET[etc[0] % 4]
                    ek = ('aET', etc[0] % 4)
                    etc[0] += 1
                    ets.append((et, ek))
                    S.op('act', lambda e, bs=bs, et=et, n=n: e.activation(out=et[:, 0:n], in_=self.ps[bs][:, 0:n], func=AF.Exp, scale=0.125),
                         r=[('ps', bs)], w=[ek])
                for m in range(2):
                    et, ek = ets[m]
                    if ATT_DUMMY:
                        S.op('pe', lambda e, kt_=kt_: e.matmul(self.ps[7][:, 0:512], lhsT=vt[:, kt_, :], rhs=dummy_rhs[:, 0:512], start=True, stop=True),
                             r=[('av', sl), 'a_dummy'], w=['ps7_dummy'])
                    S.op('pe', lambda e, m=m, et=et, kt_=kt_, n=n, ki=ki, nk=nk: e.matmul(
                        self.ps[m][:, 0:n], lhsT=vt[:, kt_, :], rhs=et[:, 0:n], start=(ki == 0), stop=(ki == nk - 1)),
                        r=[ek, ('av', sl)], w=[('ps', m)])
                    aeng = 'dve' if m == 0 else 'pool'
                    if ki == 0:
                        S.op(aeng, lambda e, m=m, et=et, n=n: e.tensor_copy(out=acc[m][:, 0:n], in_=et[:, 0:n]), r=[ek], w=[('aacc', m)])
                    else:
                        S.op(aeng, lambda e, m=m, et=et, n=n: e.tensor_tensor(out=acc[m][:, 0:n], in0=acc[m][:, 0:n], in1=et[:, 0:n], op=ALU.add),
                             r=[ek, ('aacc', m)], w=[('aacc', m)])
                if ki == nk - 1:
                    for m in range(2):
                        bz = self.bank(only=MISC)
                        S.op('pe', lambda e, bz=bz, m=m, n=n: e.matmul(self.ps[bz][:, 0:n], lhsT=self.ones[:], rhs=acc[m][:, 0:n], start=True, stop=True),
                             r=[('aacc', m), 'ones'], w=[('ps', bz)])
                        S.op('act', lambda e, bz=bz, m=m, n=n: e.activation(out=rz[m][:, 0:n], in_=self.ps[bz][:, 0:n], func=AF.Ln), r=[('ps', bz)], w=[('arz', m)])
                        S.op('act', lambda e, m=m, n=n: e.activation(out=rz[m][:, 0:n], in_=rz[m][:, 0:n], func=AF.Exp, scale=-1.0), r=[('arz', m)], w=[('arz', m)])
                        S.op('dve', lambda e, m=m, n=n: e.tensor_tensor(out=tm[m][:, 0:n], in0=self.ps[m][:, 0:n], in1=rz[m][:, 0:n], op=ALU.mult),
                             r=[('ps', m), ('arz', m)], w=[('atm', m)])
                    S.op('dve', lambda e, t0=t0, n=n: e.scalar_tensor_tensor(out=a_[:, t0:t0 + n], in0=tm[1][:, 0:n], scalar=self.NEGLAM[:, 0:1],
                                                                            in1=tm[0][:, 0:n], op0=ALU.mult, op1=ALU.add),
                         r=[('atm', 0), ('atm', 1), 'NEGLAM'], w=[('aaT', sl)])
                yield

        def chain(*gens):
            for g in gens:
                yield from g

        def interleave(main, side, ratio):
            k = 0
            side_done = False
            for _ in main:
                k += 1
                if not side_done and k % ratio == 0:
                    try:
                        next(side)
                    except StopIteration:
                        side_done = True
            if not side_done:
                for _ in side:
                    pass

        for _ in prep_gen(0):
            pass
        def zip_gens(a, b):
            da = db = False
            while not (da and db):
                if not da:
                    try:
                        next(a)
                    except StopIteration:
                        da = True
                if not db:
                    try:
                        next(b)
                    except StopIteration:
                        db = True
                yield

        def take(g, n):
            for _ in range(n):
                try:
                    next(g)
                except StopIteration:
                    return
                yield

        for h in range(8):
            sides = []
            if h >= 1:
                sides.append(out_gen(h - 1))
            if h + 1 < 8:
                sides.append(prep_gen(h + 1))
            interleave(main_gen(h), zip_gens(chain(*sides), take(pcg, 12)), 2)
        for _ in out_gen(7):
            pass
        for _ in pcg:
            pass
        self.free_phase(self.nkeep)

    def rowbcast(self, dst, l, which, ctx):
        S = self.S
        dg = self.sb("rb_diag", [128, 128], F32)
        for q in range(4):
            b = self.bank()
            for j in range(4):
                kc = q * 4 + j
                col = self.mcol(l, which, kc, ctx)
                S.op('dve', lambda e, col=col: e.tensor_scalar(out=dg[:], in0=self.ident[:], scalar1=col, scalar2=None, op0=ALU.mult),
                     r=['ident', ('M', l, which)], w=['rb_diag'])
                S.op('pe', lambda e, b=b, j=j: e.matmul(self.ps[b][:, j * 128:(j + 1) * 128], lhsT=self.ones[:], rhs=dg[:], start=True, stop=True),
                     r=['rb_diag', 'ones'], w=[('ps', b)])
            S.op('act', lambda e, b=b, q=q: e.activation(out=dst[:, q * 512:(q + 1) * 512], in_=self.ps[b][:, :], func=AF.Copy),
                 r=[('ps', b)], w=[('rb', id(dst))])
        return ('rb', id(dst))

    def phase_outproj(self, l, src, w_out, tiles):
        S = self.S
        mT = self.sb("mT", [128, KC, T], BF16)
        wo = self.sb("wo", [128, KC, D], BF16)
        for kc in range(KC):
            if kc % 4 == 0:
                q = kc // 4
                S.dma('pool', lambda e, q=q: e.dma_start(out=wo[:, q * 4:(q + 1) * 4, :],
                                                         in_=w_out[q * 512:(q + 1) * 512, :].rearrange("(k p) f -> p k f", p=128)), w=[('wo', q)])
            S.dma('sp', lambda e, kc=kc: e.dma_start(out=mT[:, kc, :], in_=self.mixT[kc * 128:(kc + 1) * 128, :]),
                  r=[('mixT', kc)], w=[('mT', kc)])
        m2b = [self.sb("m2b%d" % c, [128, D], F32) for c in range(2)]
        m2k = [self.rowbcast(m2b[c], l, 2, c) for c in range(2)]
        xts = [self.sb("op_x%d" % i, [128, D], F32) for i in range(2)]
        tmp = self.sb("op_tmp", [128, 512], F32)
        for n, i in enumerate(tiles):
            ctx = 1 if i >= TL // 128 else 0
            xt = xts[n % 2]
            xk = ('op_x', n % 2)
            S.dma('sp', lambda e, xt=xt, i=i: e.dma_start(out=xt[:], in_=src[i * 128:(i + 1) * 128, :]), r=[('xs', i)], w=[xk])
            bs_ = [self.bank() for _ in range(4)]
            for kc in range(KC):
                for db in range(4):
                    b = bs_[db]
                    S.op('pe', lambda e, b=b, kc=kc, i=i, db=db: e.matmul(self.ps[b][:, :], lhsT=mT[:, kc, i * 128:(i + 1) * 128],
                                                                          rhs=wo[:, kc, db * 512:(db + 1) * 512], start=(kc == 0), stop=(kc == KC - 1)),
                         r=[('mT', kc), ('wo', kc // 4)], w=[('ps', b)])
            for db in range(4):
                b = bs_[db]
                S.op('dve', lambda e, b=b, db=db, ctx=ctx: e.tensor_tensor(out=tmp[:], in0=self.ps[b][:, :], in1=m2b[ctx][:, db * 512:(db + 1) * 512], op=ALU.mult),
                     r=[('ps', b), m2k[ctx]], w=['op_tmp'])
                S.op('pool', lambda e, xt=xt, db=db: e.tensor_tensor(out=xt[:, db * 512:(db + 1) * 512], in0=xt[:, db * 512:(db + 1) * 512], in1=tmp[:], op=ALU.add),
                     r=['op_tmp', xk], w=[xk])
            S.dma('sp', lambda e, xt=xt, i=i: e.dma_start(out=self.xs[i * 128:(i + 1) * 128, :], in_=xt[:]), r=[xk], w=[('xs', i)])
        self.free_phase(self.nkeep)

    def precast_gen(self, l):
        S = self.S
        for e_ in range(16):
            for (src_, dst_) in ((self.moe_w1, self.w1b), (self.moe_w3, self.w3b), (self.moe_w2, self.w2b)):
                S.dma('pool', lambda e, src_=src_, dst_=dst_, e_=e_: e.dma_start(out=dst_[l, e_, :, :], in_=src_[l, e_, :, :]), w=[('wb', id(dst_), l, e_)])
                yield
                yield

    def precast_moe(self, l):
        S = self.S
        for e_ in range(16):
            for (src, dst) in ((self.moe_w1, self.w1b), (self.moe_w3, self.w3b), (self.moe_w2, self.w2b)):
                S.dma('pool', lambda e, src=src, dst=dst, e_=e_: e.dma_start(out=dst[l, e_, :, :], in_=src[l, e_, :, :]), w=[('wb', id(dst), l, e_)])

    def phase_moe(self, l, tiles, src, final=False, extra_side=None):
        S = self.S
        wr = self.sb("moe_wr", [128, KC, 20], F32)
        S.dma('sp', lambda e: e.dma_start(out=wr[:], in_=self.moe_wr[l, :, :].rearrange("(k p) e -> p k e", p=128)), w=['moe_wr'])
        br = self.sb("moe_br", [128, 20], F32)
        S.dma('sp', lambda e: e.dma_start(out=br[:], in_=self.moe_br[l:l + 1, :].partition_broadcast(128)), w=['moe_br'])
        SEL = [self.sb("moe_sel%d" % i, [16, 128], F32) for i in range(2)]
        m5b = [self.sb("m5b%d" % c, [128, D], F32) for c in range(2)]
        m5k = [self.rowbcast(m5b[c], l, 5, c) for c in range(2 if not final else 1)]
        fTs = [self.sb("moe_fT%d" % i, [128, KC, 512], BF16) for i in range(2)]
        h32 = self.sb("moe_h32", [128, KC, 128], F32)
        w1s = [self.sb("moe_w1s%d" % i, [128, KC, 512], BF16) for i in range(2)]
        w3s = [self.sb("moe_w3s%d" % i, [128, KC, 512], BF16) for i in range(2)]
        w2s = [self.sb("moe_w2s", [128, 4, D], BF16)] * 2
        yacc = self.sb("moe_yacc", [128, 4, D], F32)
        hid = self.sb("moe_hid", [128, 4, 512], BF16)
        sbuf_s = [self.sb("moe_s%d" % i, [128, 512], BF16) for i in range(2)]
        sbuf_u = [self.sb("moe_u%d" % i, [128, 512], F32) for i in range(2)]
        cbs = [self.sb("moe_cb%d" % i, [128, 512], F32) for i in range(2)]
        combTs = [self.sb("moe_combT%d" % i, [16, 512], F32) for i in range(2)]
        rt = self.sb("moe_rt", [128, 128], F32)
        xts = self.sb("moe_x", [128, D], F32)
        junk = self.sb("moe_junk", [128, D], BF16)
        wcnt = [0]
        blocks = [tiles[i:i + 4] for i in range(0, len(tiles), 4)]
        def front_gen(bi, btiles):
            fT = fTs[bi % 2]
            combT = combTs[bi % 2]
            fk = ('moe_fT', bi % 2)
            ck = ('combT', bi % 2)
            for ti, i in enumerate(btiles):
                yield from self._nmt_tile(src, i, l, 1, fT, ti, h32, xts, junk, rt, fk)
                b = 7
                for kc in range(KC):
                    S.op('pe', lambda e, b=b, kc=kc: e.matmul(self.ps[b][:, 0:20], lhsT=h32[:, kc, :], rhs=wr[:, kc, :], start=(kc == 0), stop=(kc == KC - 1)),
                         r=['moe_h32', 'moe_wr'], w=[('ps', b)])
                yield
                lg = rt[:, 8:28]
                S.op('dve', lambda e, b=b, lg=lg: e.tensor_tensor(out=lg, in0=self.ps[b][:, 0:20], in1=br[:], op=ALU.add), r=[('ps', b), 'moe_br'], w=['rt_lg'])
                gmax, ngmax, gsum, gw = rt[:, 28:29], rt[:, 29:30], rt[:, 30:31], rt[:, 31:32]
                gmask, pen = rt[:, 32:36], rt[:, 36:40]
                elm, msk1, elm2, msk2 = rt[:, 40:56], rt[:, 56:72], rt[:, 72:88], rt[:, 96:112]
                m1, m2_, dd, w1g, w2g = rt[:, 88:89], rt[:, 89:90], rt[:, 90:91], rt[:, 91:92], rt[:, 92:93]
                gjunk = rt[:, 4:8]
                k_ = 'rt'
                S.op('dve', lambda e: e.tensor_reduce(out=gmax, in_=lg[:, 0:4], axis=AX.X, op=ALU.max), r=['rt_lg'], w=[k_ + '1'])
                S.op('dve', lambda e: e.tensor_scalar(out=ngmax, in0=gmax, scalar1=-1.0, scalar2=None, op0=ALU.mult), r=[k_ + '1'], w=[k_ + '2'])
                S.op('act', lambda e: e.activation(out=gjunk, in_=lg[:, 0:4], func=AF.Exp, bias=ngmax, scale=1.0, accum_out=gsum), r=['rt_lg', k_ + '2'], w=[k_ + '3'])
                S.op('dve', lambda e: e.reciprocal(out=gw, in_=gsum), r=[k_ + '3'], w=[k_ + '4'])
                S.op('dve', lambda e: e.tensor_scalar(out=gmask, in0=lg[:, 0:4], scalar1=gmax, scalar2=None, op0=ALU.is_ge), r=['rt_lg', k_ + '1'], w=[k_ + '5'])
                S.op('dve', lambda e: e.tensor_scalar(out=pen, in0=gmask, scalar1=-1.0, scalar2=1e30, op0=ALU.add, op1=ALU.mult), r=[k_ + '5'], w=[k_ + '6'])
                S.op('dve', lambda e: e.tensor_tensor(out=elm.rearrange("p (g j) -> p g j", j=4), in0=lg[:, 4:20].rearrange("p (g j) -> p g j", j=4),
                                                     in1=pen.unsqueeze(2).to_broadcast([128, 4, 4]), op=ALU.add), r=['rt_lg', k_ + '6'], w=[k_ + '7'])
                S.op('dve', lambda e: e.tensor_reduce(out=m1, in_=elm, axis=AX.X, op=ALU.max), r=[k_ + '7'], w=[k_ + '8'])
                S.op('dve', lambda e: e.tensor_scalar(out=msk1, in0=elm, scalar1=m1, scalar2=None, op0=ALU.is_ge), r=[k_ + '7', k_ + '8'], w=[k_ + '9'])
                S.op('dve', lambda e: e.scalar_tensor_tensor(out=elm2, in0=msk1, scalar=-1e30, in1=elm, op0=ALU.mult, op1=ALU.add), r=[k_ + '9', k_ + '7'], w=[k_ + '10'])
                S.op('dve', lambda e: e.tensor_reduce(out=m2_, in_=elm2, axis=AX.X, op=ALU.max), r=[k_ + '10'], w=[k_ + '11'])
                S.op('dve', lambda e: e.tensor_scalar(out=msk2, in0=elm2, scalar1=m2_, scalar2=None, op0=ALU.is_ge), r=[k_ + '10', k_ + '11', 'rt_lg'], w=[k_ + '12'])
                S.op('dve', lambda e: e.tensor_tensor(out=dd, in0=m2_, in1=m1, op=ALU.subtract), r=[k_ + '11', k_ + '8'], w=[k_ + '13'])
                S.op('act', lambda e: e.activation(out=dd, in_=dd, func=AF.Exp), r=[k_ + '13'], w=[k_ + '13'])
                S.op('dve', lambda e: e.tensor_scalar(out=w1g, in0=dd, scalar1=1.0, scalar2=None, op0=ALU.add), r=[k_ + '13'], w=[k_ + '14'])
                S.op('dve', lambda e: e.reciprocal(out=w1g, in_=w1g), r=[k_ + '14'], w=[k_ + '14'])
                S.op('dve', lambda e: e.tensor_tensor(out=w1g, in0=w1g, in1=gw, op=ALU.mult), r=[k_ + '14', k_ + '4'], w=[k_ + '14'])
                S.op('dve', lambda e: e.tensor_tensor(out=w2g, in0=w1g, in1=dd, op=ALU.mult), r=[k_ + '14', k_ + '13'], w=[k_ + '15'])
                S.op('dve', lambda e: e.tensor_scalar(out=msk1, in0=msk1, scalar1=w1g, scalar2=None, op0=ALU.mult), r=[k_ + '9', k_ + '14'], w=[k_ + '9'])
                S.op('dve', lambda e: e.scalar_tensor_tensor(out=elm, in0=msk2, scalar=w2g, in1=msk1, op0=ALU.mult, op1=ALU.add),
                     r=[k_ + '12', k_ + '15', k_ + '9'], w=[k_ + '7'])
                yield
                b2 = self.bank()
                S.op('pe', lambda e, b2=b2: e.transpose(out=self.ps[b2][0:16, 0:128], in_=elm, identity=self.ident[:]), r=[k_ + '7', 'ident'], w=[('ps', b2)])
                S.op('act', lambda e, b2=b2, ti=ti: e.activation(out=combT[:, ti * 128:(ti + 1) * 128], in_=self.ps[b2][0:16, 0:128], func=AF.Copy),
                     r=[('ps', b2)], w=[ck])
                yield

        def experts_gen(bi, btiles):
            nb = len(btiles) * 128
            ctx = 1 if btiles[0] >= TL // 128 else 0
            fT = fTs[bi % 2]
            combT = combTs[bi % 2]
            fk = ('moe_fT', bi % 2)
            ck = ('combT', bi % 2)
            def emit_comb(e_):
                bc = self.bank()
                sel = SEL[e_ % 2]
                cbb = cbs[e_ % 2]
                S.op('dve', lambda e, sel=sel, e_=e_: e.tensor_copy(out=sel[:], in_=self.ident[0:16, e_:e_ + 1].to_broadcast([16, 128])),
                     r=['ident'], w=[('SEL', e_ % 2)])
                S.op('pe', lambda e, bc=bc, sel=sel, nb=nb: e.matmul(self.ps[bc][:, 0:nb], lhsT=sel[:], rhs=combT[:, 0:nb], start=True, stop=True),
                     r=[('SEL', e_ % 2), ck], w=[('ps', bc)])
                S.op('act', lambda e, bc=bc, nb=nb, cbb=cbb: e.activation(out=cbb[:, 0:nb], in_=self.ps[bc][:, 0:nb], func=AF.Copy), r=[('ps', bc)], w=[('moe_cb', e_ % 2)])

            for e_ in range(16):
                wi = wcnt[0] % 2
                wcnt[0] += 1
                cb = cbs[e_ % 2]
                for half in range(2):
                    S.dma('sp', lambda e, wi=wi, e_=e_, half=half: e.dma_start(out=w1s[wi][:, half * 8:(half + 1) * 8, :],
                          in_=self.w1b[l, e_, half * 1024:(half + 1) * 1024, :].rearrange("(k p) f -> p k f", p=128)),
                          r=[('wb', id(self.w1b), l, e_)], w=[('w1s', wi)])
                    S.dma('sp', lambda e, wi=wi, e_=e_, half=half: e.dma_start(out=w3s[wi][:, half * 8:(half + 1) * 8, :],
                          in_=self.w3b[l, e_, half * 1024:(half + 1) * 1024, :].rearrange("(k p) f -> p k f", p=128)),
                          r=[('wb', id(self.w3b), l, e_)], w=[('w3s', wi)])
                S.dma('sp', lambda e, wi=wi, e_=e_: e.dma_start(out=w2s[wi][:], in_=self.w2b[l, e_, :, :].rearrange("(k p) f -> p k f", p=128)),
                      r=[('wb', id(self.w2b), l, e_)], w=[('w2s', 0)])
                if e_ == 0:
                    emit_comb(0)
                for j in range(4):
                    b1 = self.bank()
                    b3 = self.bank()
                    for (bb, ws, wk) in ((b1, w1s[wi], ('w1s', wi)), (b3, w3s[wi], ('w3s', wi))):
                        for kc in range(KC):
                            S.op('pe', lambda e, bb=bb, ws=ws, kc=kc, j=j, nb=nb: e.matmul(
                                self.ps[bb][:, 0:nb], lhsT=ws[:, kc, j * 128:(j + 1) * 128], rhs=fT[:, kc, 0:nb], start=(kc == 0), stop=(kc == KC - 1)),
                                r=[wk, fk], w=[('ps', bb)])
                    ss, uu = sbuf_s[j % 2], sbuf_u[j % 2]
                    S.op('act', lambda e, b1=b1, ss=ss, nb=nb: e.activation(out=ss[:, 0:nb], in_=self.ps[b1][:, 0:nb], func=AF.Silu),
                         r=[('ps', b1)], w=[('moe_s', j % 2)])
                    S.op('dve', lambda e, b3=b3, uu=uu, nb=nb, cb=cb: e.tensor_tensor(out=uu[:, 0:nb], in0=self.ps[b3][:, 0:nb], in1=cb[:, 0:nb], op=ALU.mult),
                         r=[('ps', b3), ('moe_cb', e_ % 2)], w=[('moe_u', j % 2)])
                    S.op('pool', lambda e, ss=ss, uu=uu, j=j, nb=nb: e.tensor_tensor(out=hid[:, j, 0:nb], in0=ss[:, 0:nb], in1=uu[:, 0:nb], op=ALU.mult),
                         r=[('moe_s', j % 2), ('moe_u', j % 2)], w=[('moe_hid', j)])
                    if j == 2 and e_ + 1 < 16:
                        emit_comb(e_ + 1)
                    yield
                for ti in range(len(btiles)):
                    for db in range(4):
                        b = self.bank()
                        for j in range(4):
                            S.op('pe', lambda e, b=b, j=j, ti=ti, db=db, wi=wi: e.matmul(
                                self.ps[b][:, :], lhsT=hid[:, j, ti * 128:(ti + 1) * 128], rhs=w2s[wi][:, j, db * 512:(db + 1) * 512],
                                start=(j == 0), stop=(j == 3)), r=[('moe_hid', j), ('w2s', 0)], w=[('ps', b)])
                        ya = yacc[:, ti, db * 512:(db + 1) * 512]
                        yk = ('yacc', ti, db)
                        if e_ == 0:
                            S.op('act', lambda e, b=b, ya=ya: e.activation(out=ya, in_=self.ps[b][:, :], func=AF.Copy), r=[('ps', b)], w=[yk])
                        else:
                            S.op('dve', lambda e, b=b, ya=ya: e.tensor_tensor(out=ya, in0=self.ps[b][:, :], in1=ya, op=ALU.add), r=[('ps', b), yk], w=[yk])
            for ti, i in enumerate(btiles):
                S.dma('sp', lambda e, i=i: e.dma_start(out=xts[:], in_=src[i * 128:(i + 1) * 128, :]), r=[('xs', i)], w=['moe_x'])
                for db in range(4):
                    S.op('dve', lambda e, ti=ti, db=db, ctx=ctx: e.tensor_tensor(out=yacc[:, ti, db * 512:(db + 1) * 512], in0=yacc[:, ti, db * 512:(db + 1) * 512],
                                                                            in1=m5b[ctx][:, db * 512:(db + 1) * 512], op=ALU.mult),
                         r=[('yacc', ti, db), m5k[ctx]], w=[('yacc', ti, db)])
                    S.op('pool', lambda e, ti=ti, db=db: e.tensor_tensor(out=yacc[:, ti, db * 512:(db + 1) * 512], in0=yacc[:, ti, db * 512:(db + 1) * 512],
                                                                      in1=xts[:, db * 512:(db + 1) * 512], op=ALU.add),
                         r=[('yacc', ti, db), 'moe_x'], w=[('yacc', ti, db)])
                dst = self.out if final else self.xs
                S.dma('sp', lambda e, dst=dst, ti=ti, i=i: e.dma_start(out=dst[i * 128:(i + 1) * 128, :], in_=yacc[:, ti, :]),
                      r=[('yacc', ti, db) for db in range(4)], w=[('xs', i) if not final else ('out', i)])
            yield

        def interleave(main, side):
            side_done = side is None
            for _ in main:
                if not side_done:
                    try:
                        next(side)
                    except StopIteration:
                        side_done = True
            if not side_done:
                for _ in side:
                    pass

        self.bank_reserved = (7,)
        for _ in front_gen(0, blocks[0]):
            pass
        def chain2(a, b, n):
            if a is not None:
                yield from a
            if b is not None:
                for _ in range(n):
                    try:
                        next(b)
                    except StopIteration:
                        return
                    yield

        for bi, btiles in enumerate(blocks):
            side = front_gen(bi + 1, blocks[bi + 1]) if bi + 1 < len(blocks) else None
            interleave(experts_gen(bi, btiles), chain2(side, extra_side, 40))
        if extra_side is not None:
            for _ in extra_side:
                pass
        self.bank_reserved = ()
        self.free_phase(self.nkeep)

    def _nmt_tile(self, src, i, l, nrm, hT, ti, h32, xt, junk, st, fk):
        S = self.S
        shidx = 0 if nrm == 0 else 3
        ctx = 1 if i >= TL // 128 else 0
        S.dma('sp', lambda e: e.dma_start(out=xt[:], in_=src[i * 128:(i + 1) * 128, :]), r=[('xs', i)], w=['moe_x'])
        ss, rs = st[:, 0:1], st[:, 1:2]
        S.op('act', lambda e: e.activation(out=junk[:], in_=xt[:], func=AF.Square, accum_out=ss), r=['moe_x'], w=['nmt_ss', 'moe_junk'])
        S.op('act', lambda e: e.activation(out=ss, in_=ss, func=AF.Sqrt, bias=self.epsc[:, 0:1], scale=1.0 / D), r=['nmt_ss', 'epsc'], w=['nmt_ss'])
        S.op('dve', lambda e: e.reciprocal(out=rs, in_=ss), r=['nmt_ss'], w=['nmt_rs'])
        S.op('dve', lambda e: e.tensor_scalar(out=xt[:], in0=xt[:], scalar1=rs, scalar2=None, op0=ALU.mult), r=['nmt_rs', 'moe_x'], w=['moe_x'])
        yield
        gs = self.GS[(l, nrm, ctx)]
        for q in range(4):
            b = self.bank()
            for j in range(4):
                kc = q * 4 + j
                S.op('pe', lambda e, b=b, j=j, kc=kc: e.transpose(out=self.ps[b][:, j * 128:(j + 1) * 128], in_=xt[:, kc * 128:(kc + 1) * 128],
                                                                  identity=self.ident[:]), r=['moe_x', 'ident'], w=[('ps', b)])
            for j in range(4):
                kc = q * 4 + j
                sh = self.mcol(l, shidx, kc, ctx)
                if j % 2 == 0:
                    S.op('act', lambda e, b=b, j=j, kc=kc, sh=sh: e.activation(out=h32[:, kc, :], in_=self.ps[b][:, j * 128:(j + 1) * 128], func=AF.Identity,
                                                                              bias=sh, scale=gs[:, kc:kc + 1]), r=[('ps', b), ('GS', l, nrm, ctx), ('M', l, shidx)], w=['moe_h32'])
                else:
                    S.op('dve', lambda e, b=b, j=j, kc=kc, sh=sh: e.tensor_scalar(out=h32[:, kc, :], in0=self.ps[b][:, j * 128:(j + 1) * 128],
                                                                                 scalar1=gs[:, kc:kc + 1], scalar2=sh, op0=ALU.mult, op1=ALU.add),
                         r=[('ps', b), ('GS', l, nrm, ctx), ('M', l, shidx)], w=['moe_h32'])
        yield
        S.op('pool', lambda e: e.tensor_copy(out=hT[:, :, ti * 128:(ti + 1) * 128], in_=h32[:, :, :]), r=['moe_h32'], w=[fk])

    def phase_inproj1(self):
        S = self.S
        hT = self.sb("hT1", [128, KC, T], BF16)
        self.nmt(self.xs, list(range(NT)), 1, 0, hT, tag="b")
        stg = [self.sb("stg1_%d" % i, [128, T], F32) for i in range(2)]
        tmp = self.sb("stg1_tmp", [128, T], F32)
        cnt = [0]

        def evac(g, t0, n, pap, b):
            st = stg[cnt[0] % 2]
            S.op('act', lambda e, st=st, t0=t0, n=n, pap=pap: e.activation(out=st[:, t0:t0 + n], in_=pap, func=AF.Copy),
                 r=[('ps', b)], w=[('stg', cnt[0] % 2)])

        def done(g):
            st = stg[cnt[0] % 2]
            sk = ('stg', cnt[0] % 2)
            if g < 16:
                S.op('act', lambda e, st=st: e.activation(out=tmp[:], in_=st[:], func=AF.Square), r=[sk], w=['stg_tmp'])
                S.op('dve', lambda e: e.tensor_scalar(out=tmp[:], in0=tmp[:], scalar1=0.044715, scalar2=1.0, op0=ALU.mult, op1=ALU.add),
                     r=['stg_tmp'], w=['stg_tmp'])
                S.op('pool', lambda e, st=st: e.tensor_tensor(out=tmp[:], in0=tmp[:], in1=st[:], op=ALU.mult), r=['stg_tmp', sk], w=['stg_tmp'])
                S.op('act', lambda e: e.activation(out=tmp[:], in_=tmp[:], func=AF.Sigmoid, scale=2.0 * math.sqrt(2.0 / math.pi)),
                     r=['stg_tmp'], w=['stg_tmp'])
                S.op('dve', lambda e, st=st: e.tensor_tensor(out=st[:], in0=st[:], in1=tmp[:], op=ALU.mult), r=['stg_tmp', sk], w=[sk])
            S.dma('sp', lambda e, st=st, g=g: e.dma_start(out=self.projT[g * 128:(g + 1) * 128, :], in_=st[:]), r=[sk], w=[('projT', g)])
            cnt[0] += 1

        groups = [[q * 4 + j for j in range(4)] for q in range(8)]
        self.linear_fm(self.w_in1, groups, hT, "b", evac, done)
        self.free_phase(self.nkeep)

    def phase_rglru(self):
        S = self.S
        GW = self.sb("rg_GW", [128, 64, 128], BF16)
        for q in range(4):
            S.dma('pool', lambda e, q=q: e.dma_start(out=GW[:, q * 16:(q + 1) * 16, :],
                                                     in_=self.gate_w[q // 2, q % 2, :, :, :].rearrange("h i j -> i h j")), w=[('GW', q)])
        CL = self.sb("rg_CL", [128, 32], F32)
        S.op('act', lambda e: e.activation(out=CL[:], in_=self.pc('rg_lam'), func=AF.Exp, scale=-1.0), r=['PC'], w=['CL'])
        S.op('act', lambda e: e.activation(out=CL[:], in_=CL[:], func=AF.Ln, bias=self.ones[:, 0:1], scale=1.0), r=['CL', 'ones'], w=['CL'])
        S.op('dve', lambda e: e.tensor_scalar(out=CL[:], in0=CL[:], scalar1=-8.0, scalar2=None, op0=ALU.mult), r=['CL'], w=['CL'])
        ygs = [self.sb("rg_y%d" % i, [128, T], F32) for i in range(2)]
        us = [self.sb("rg_u%d" % i, [128, T], F32) for i in range(2)]
        uc = self.sb("rg_uc", [128, T], F32)
        ucb = self.sb("rg_ucb", [128, T], BF16)
        rr = self.sb("rg_r", [128, T], F32)
        ig = self.sb("rg_i", [128, T], F32)
        a = self.sb("rg_a", [128, T], F32)
        tmp = self.sb("rg_tmp", [128, T], F32)
        bx = self.sb("rg_bx", [128, T], F32)
        hh = [self.sb("rg_h%d" % i, [128, T], F32) for i in range(2)]
        ob = self.sb("rg_ob", [128, TL], BF16)
        segs = [(0, TL), (TL, T)]
        o_cw, _ = PV_OFF['conv_w']
        o_cb, _ = PV_OFF['conv_b']
        o_gb, _ = PV_OFF['gate_b']
        def load(j):
            yg_, u_ = ygs[j % 2], us[j % 2]
            S.dma('sp', lambda e: e.dma_start(out=yg_[:], in_=self.projT[j * 128:(j + 1) * 128, :]), r=[('projT', j)], w=[('rg_y', j % 2)])
            S.dma('sp', lambda e: e.dma_start(out=u_[:], in_=self.projT[(16 + j) * 128:(17 + j) * 128, :]), r=[('projT', 16 + j)], w=[('rg_u', j % 2)])

        def chunk(j, yg, u):
            yk, uk = ('rg_y', j % 2), ('rg_u', j % 2)
            if j + 1 < 16:
                load(j + 1)
            wcol = lambda i, j=j: self.PC[:, o_cw + i * 16 + j:o_cw + i * 16 + j + 1]
            bcol = self.PC[:, o_cb + j:o_cb + j + 1]
            S.op('act', lambda e, wc=wcol(2), bcol=bcol: e.activation(out=uc[:], in_=u[:], func=AF.Identity, bias=bcol, scale=wc), r=[uk, 'PC'], w=['rg_uc'])
            for (s0, s1) in segs:
                for (tap, sh) in ((0, -2), (1, -1), (3, 1)):
                    if sh < 0:
                        o0, o1, i0, i1 = s0 - sh, s1, s0, s1 + sh
                    else:
                        o0, o1, i0, i1 = s0, s1 - sh, s0 + sh, s1
                    S.op('dve', lambda e, wc=wcol(tap), o0=o0, o1=o1, i0=i0, i1=i1: e.scalar_tensor_tensor(
                        out=uc[:, o0:o1], in0=u[:, i0:i1], scalar=wc, in1=uc[:, o0:o1], op0=ALU.mult, op1=ALU.add), r=[uk, 'rg_uc', 'PC'], w=['rg_uc'])
            S.op('act', lambda e: e.activation(out=ucb[:], in_=uc[:], func=AF.Copy), r=['rg_uc'], w=['rg_ucb'])
            for d_ in range(2):
                for g_ in range(2):
                    dst = rr if g_ == 0 else ig
                    dk = 'rg_r' if g_ == 0 else 'rg_i'
                    idx = (d_ * 2 + g_) * 16 + j
                    gb = self.PC[:, o_gb + idx:o_gb + idx + 1]
                    for (t0, n) in self.tblocks(0, T):
                        b = self.bank()
                        S.op('pe', lambda e, b=b, idx=idx, t0=t0, n=n: e.matmul(self.ps[b][:, 0:n], lhsT=GW[:, idx, :], rhs=ucb[:, t0:t0 + n], start=True, stop=True),
                             r=[('GW', idx // 16), 'rg_ucb'], w=[('ps', b)])
                        S.op('act', lambda e, b=b, dst=dst, gb=gb, t0=t0, n=n: e.activation(out=dst[:, t0:t0 + n], in_=self.ps[b][:, 0:n], func=AF.Sigmoid, bias=gb, scale=1.0),
                             r=[('ps', b), 'PC'], w=[dk])
                clc = CL[:, d_ * 16 + j:d_ * 16 + j + 1]
                S.op('act', lambda e, clc=clc: e.activation(out=a[:], in_=rr[:], func=AF.Exp, scale=clc), r=['rg_r', 'CL'], w=['rg_a'])
                S.op('act', lambda e: e.activation(out=tmp[:], in_=a[:], func=AF.Square), r=['rg_a'], w=['rg_tmp'])
                S.op('act', lambda e: e.activation(out=tmp[:], in_=tmp[:], func=AF.Sqrt, bias=self.ones[:, 0:1], scale=-1.0), r=['rg_tmp', 'ones'], w=['rg_tmp'])
                S.op('dve', lambda e: e.tensor_tensor(out=bx[:], in0=tmp[:], in1=ig[:], op=ALU.mult), r=['rg_tmp', 'rg_i'], w=['rg_bx'])
                S.op('pool', lambda e: e.tensor_tensor(out=bx[:], in0=bx[:], in1=uc[:], op=ALU.mult), r=['rg_bx', 'rg_uc'], w=['rg_bx'])
                h_ = hh[d_]
                hk = ('rg_h', d_)
                if d_ == 0:
                    S.op('dve', lambda e, h_=h_: e.tensor_tensor_scan(out=h_[:, TL:T], data0=a[:, TL:T], data1=bx[:, TL:T], initial=0.0, op0=ALU.mult, op1=ALU.add),
                         r=['rg_a', 'rg_bx'], w=[hk])
                    S.op('dve', lambda e, h_=h_: e.tensor_tensor_scan(out=h_[:, 0:TL], data0=a[:, 0:TL], data1=bx[:, 0:TL], initial=h_[:, T - 1:T], op0=ALU.mult, op1=ALU.add),
                         r=['rg_a', 'rg_bx', hk], w=[hk])
                else:
                    S.op('dve', lambda e, h_=h_: e.tensor_tensor_scan(out=h_[:, TL:T][:, ::-1], data0=a[:, TL:T][:, ::-1], data1=bx[:, TL:T][:, ::-1], initial=0.0,
                                                                     op0=ALU.mult, op1=ALU.add), r=['rg_a', 'rg_bx'], w=[hk])
                    S.op('dve', lambda e, h_=h_: e.tensor_tensor_scan(out=h_[:, 0:TL][:, ::-1], data0=a[:, 0:TL][:, ::-1], data1=bx[:, 0:TL][:, ::-1],
                                                                     initial=h_[:, TL:TL + 1], op0=ALU.mult, op1=ALU.add), r=['rg_a', 'rg_bx', hk], w=[hk])
            S.op('pool', lambda e: e.tensor_tensor(out=tmp[:, 0:TL], in0=hh[0][:, 0:TL], in1=hh[1][:, 0:TL], op=ALU.add), r=[('rg_h', 0), ('rg_h', 1)], w=['rg_tmp'])
            S.op('dve', lambda e: e.tensor_tensor(out=ob[:], in0=tmp[:, 0:TL], in1=yg[:, 0:TL], op=ALU.mult), r=['rg_tmp', yk], w=['rg_ob'])
            S.dma('sp', lambda e, j=j: e.dma_start(out=self.mixT[j * 128:(j + 1) * 128, 0:TL], in_=ob[:]), r=['rg_ob'], w=[('mixT', j)])
        load(0)
        for j in range(16):
            chunk(j, ygs[j % 2], us[j % 2])
        self.free_phase(self.nkeep)

    def build(self):
        self.phase_prep()
        if self.upto >= 1:
            self.phase_inproj0()
        if self.upto >= 2:
            self.phase_hgrn()
        if self.upto >= 3:
            self.phase_attn()
        if self.upto >= 4:
            self.phase_outproj(0, self.xin, self.w_out0, list(range(NT)))
        if self.upto >= 5:
            self.phase_moe(0, list(range(NT)), self.xs, extra_side=self.precast_gen(1))
        if self.upto >= 6:
            self.phase_inproj1()
        if self.upto >= 7:
            self.phase_rglru()
        if self.upto >= 8:
            self.phase_outproj(1, self.xs, self.w_out1, list(range(TL // 128)))
        if self.upto >= 9:
            self.phase_moe(1, list(range(TL // 128)), self.xs, final=True)
        if self.debug:
            S = self.S
            S.op('dve', lambda e: e.tensor_copy(out=self.dbgt[:, 0:192], in_=self.M[0][:].rearrange("p f t -> p (f t)")),
                 r=[('M', 0)], w=['dbgt']) if False else None
        n = self.S.finalize()
        return n


def make_in_maps(inp, cores):
    consts = host_consts()
    shared = {
        'ada_w': np.ascontiguousarray(inp['ada_w'], np.float32),
        'w_in0': np.ascontiguousarray(inp['even_w_in'][0], np.float32),
        'w_out0': np.ascontiguousarray(inp['even_w_out'][0], np.float32),
        'w_in1': np.ascontiguousarray(inp['odd_w_in'][0], np.float32),
        'w_out1': np.ascontiguousarray(inp['odd_w_out'][0], np.float32),
        'gate_w': np.ascontiguousarray(inp['rg_gate_w'][0], np.float32),
        'moe_wr': np.ascontiguousarray(np.concatenate([inp['moe_w_grp'], inp['moe_w_exp']], axis=-1), np.float32),
        'moe_br': np.ascontiguousarray(np.concatenate([inp['moe_b_grp'], inp['moe_b_exp']], axis=-1), np.float32),
        'moe_w1': np.ascontiguousarray(inp['moe_w1'], np.float32),
        'moe_w3': np.ascontiguousarray(inp['moe_w3'], np.float32),
        'moe_w2': np.ascontiguousarray(inp['moe_w2'], np.float32),
    }
    for k, v in consts.items():
        shared['c_' + k] = v
    maps = []
    for b in cores:
        m = dict(shared)
        m['xin'] = np.ascontiguousarray(np.concatenate([inp['x'][b], inp['ctx'][b]], axis=0), np.float32)
        m['pvec'] = pack_pvec(inp, b)
        maps.append(m)
    return maps


def run(inp, debug=False, upto=99, cores=None):
    if cores is None:
        cores = list(range(NCORES))
    p = Prog(debug=debug, upto=upto)
    import time as _t
    _t0 = _t.time()
    n = p.build()
    print("instructions:", n, "build_s", round(_t.time() - _t0, 1), flush=True)
    maps = make_in_maps(inp, cores)
    _t0 = _t.time()
    res = run_bass_kernel_spmd(p.nc, maps, core_ids=list(range(len(cores))))
    print("compile+run_s", round(_t.time() - _t0, 1), flush=True)
    return res.results


def kernel(**inputs):
    inp = {k: np.asarray(v) for k, v in inputs.items()}
    res = run(inp)
    out = np.stack([np.asarray(r['out'], np.float32) for r in res], axis=0)
    return out
```
